# Optimizing a Trainium2 kernel written in Bass

```python
import jax, jax.numpy as jnp
from jax import lax
import numpy as np

D_MODEL = 1024
BATCH = 8
SEQ = 4096
DEPTH = 4

D_MIX = D_MODEL
N_GROUPS = 4
GROUP_WIDTH = D_MIX // N_GROUPS
HEAD_DIM = 64
N_HEADS_GROUP = GROUP_WIDTH // HEAD_DIM
CONF_KERNEL = 31
SHORT_KERNEL = 3
FFN_KERNEL = 3
D_FF = 2816
BLOCK_Q = 128
RET_CHUNK = 128
ROPE_BASE = 10000.0
NORM_EPS = 1e-6
N_IN_SLICES = 12
D_IN = N_IN_SLICES * GROUP_WIDTH

kernel_name = 'hybrid_parallel_heads_stickbreak_retention_conv'


def rms_norm(x, g):
    xf = x.astype(jnp.float32)
    y = xf * lax.rsqrt(jnp.mean(xf * xf, axis=-1, keepdims=True) + NORM_EPS)
    return (y * g.astype(jnp.float32)).astype(x.dtype)


def layer_norm(x, g, b):
    xf = x.astype(jnp.float32)
    mu = jnp.mean(xf, axis=-1, keepdims=True)
    xc = xf - mu
    y = xc * lax.rsqrt(jnp.mean(xc * xc, axis=-1, keepdims=True) + NORM_EPS)
    return (y * g.astype(jnp.float32) + b.astype(jnp.float32)).astype(x.dtype)


def causal_dwconv(x, w):
    k_w, c = w.shape
    return lax.conv_general_dilated(
        x, w[:, None, :].astype(x.dtype), window_strides=(1,), padding=[(k_w - 1, 0)],
        dimension_numbers=('NWC', 'WIO', 'NWC'), feature_group_count=c)


def split_heads(x):
    b, t, _ = x.shape
    return x.reshape(b, t, -1, HEAD_DIM).transpose(0, 2, 1, 3)


def merge_heads(x):
    b, h, t, d = x.shape
    return x.transpose(0, 2, 1, 3).reshape(b, t, h * d)


def rotary(x, pos):
    d = x.shape[-1]
    half = d // 2
    inv_freq = ROPE_BASE ** (-jnp.arange(0, d, 2, dtype=jnp.float32) / d)
    ang = pos.astype(jnp.float32)[:, None] * inv_freq[None, :]
    cos, sin = jnp.cos(ang), jnp.sin(ang)
    xf = x.astype(jnp.float32)
    x1, x2 = xf[..., :half], xf[..., half:]
    return jnp.concatenate([x1 * cos - x2 * sin, x2 * cos + x1 * sin], axis=-1)


def conformer_conv(a, gate, dw_w, dw_b, ln_g, ln_b):
    h = a * jax.nn.sigmoid(gate)
    h = causal_dwconv(h, dw_w) + dw_b.astype(h.dtype)
    return jax.nn.silu(layer_norm(h, ln_g, ln_b))


def stick_breaking_attention(q, k, v):
    b, h, t, d = q.shape
    n_blocks = t // BLOCK_Q
    scale = d ** -0.5
    kf = k.astype(jnp.float32)
    key_pos = jnp.arange(t)
    qb = q.reshape(b, h, n_blocks, BLOCK_Q, d).transpose(2, 0, 1, 3, 4)

    def block(args):
        qi, i = args
        z = jnp.einsum('bhqd,bhkd->bhqk', qi.astype(jnp.float32), kf) * scale
        q_pos = i * BLOCK_Q + jnp.arange(BLOCK_Q)
        mask = key_pos[None, :] < q_pos[:, None]
        log_1m_beta = jnp.where(mask, -jax.nn.softplus(z), 0.0)
        tail = lax.cumsum(log_1m_beta, axis=3, reverse=True) - log_1m_beta
        attn = jnp.where(mask, jnp.exp(jax.nn.log_sigmoid(z) + tail), 0.0)
        return jnp.einsum('bhqk,bhkd->bhqd', attn.astype(v.dtype), v)

    out = lax.map(block, (qb, jnp.arange(n_blocks)))
    return out.transpose(1, 2, 0, 3, 4).reshape(b, h, t, d)


def retention_chunkwise(q, k, v):
    b, h, t, d = q.shape
    n_chunks = t // RET_CHUNK
    log_gamma = jnp.log1p(-jnp.exp2(-5.0 - jnp.arange(h, dtype=jnp.float32)))
    idx = jnp.arange(RET_CHUNK, dtype=jnp.float32)
    diff = idx[:, None] - idx[None, :]
    causal = diff >= 0
    d_intra = jnp.where(causal[None], jnp.exp(jnp.where(causal, diff, 0.0)[None] * log_gamma[:, None, None]), 0.0)
    q_decay = jnp.exp((idx[None, :] + 1.0) * log_gamma[:, None])
    k_decay = jnp.exp((RET_CHUNK - 1.0 - idx[None, :]) * log_gamma[:, None])
    chunk_decay = jnp.exp(RET_CHUNK * log_gamma)
    k = k * (d ** -0.5)

    def to_chunks(z):
        return z.reshape(b, h, n_chunks, RET_CHUNK, d).transpose(2, 0, 1, 3, 4)

    def step(state, inp):
        qc, kc, vc = inp
        inner = jnp.einsum('bhqd,bhkd->bhqk', qc, kc) * d_intra
        o = jnp.einsum('bhqk,bhkd->bhqd', inner, vc) + jnp.einsum('bhqd,bhde->bhqe', qc, state) * q_decay[..., None]
        state = state * chunk_decay[:, None, None] + jnp.einsum('bhkd,bhke->bhde', kc * k_decay[..., None], vc)
        return state, o

    state0 = jnp.zeros((b, h, d, d), jnp.float32)
    _, out = lax.scan(step, state0, (to_chunks(q), to_chunks(k), to_chunks(v)))
    return out.transpose(1, 2, 0, 3, 4).reshape(b, h, t, d)


def retention_mixer(q, k, v, g, norm_g):
    t = q.shape[1]
    pos = jnp.arange(t)
    qh = rotary(split_heads(q), pos)
    kh = rotary(split_heads(k), pos)
    vh = split_heads(v).astype(jnp.float32)
    o = retention_chunkwise(qh, kh, vh)
    mu = jnp.mean(o, axis=-1, keepdims=True)
    oc = o - mu
    o = oc * lax.rsqrt(jnp.mean(oc * oc, axis=-1, keepdims=True) + NORM_EPS)
    y = merge_heads(o) * norm_g.astype(jnp.float32)
    return (jax.nn.silu(g.astype(jnp.float32)) * y).astype(q.dtype)


def short_gated_conv(b_gate, c_gate, h, conv_w):
    return b_gate * causal_dwconv(c_gate * h, conv_w)


def conv_ffn(x, w_up, w_conv, w_down):
    h = causal_dwconv(x @ w_up, w_conv)
    gate, up = jnp.split(h, 2, axis=-1)
    return (jax.nn.silu(gate) * up) @ w_down


def setup_inputs(seed: int = 0) -> dict:
    key = jax.random.key(seed)
    ks = jax.random.split(key, 16)
    f32 = jnp.float32

    def nrm(k, shape, scale):
        return jax.random.normal(k, shape, f32) * scale

    def gain(k, shape):
        return 1.0 + 0.05 * jax.random.normal(k, shape, f32)

    return {
        'x': nrm(ks[0], (BATCH, SEQ, D_MODEL), 1.0),
        'norm_mix_pre': gain(ks[1], (DEPTH, D_MODEL)),
        'norm_mix_post': gain(ks[2], (DEPTH, D_MODEL)),
        'norm_ffn_pre': gain(ks[3], (DEPTH, D_MODEL)),
        'norm_ffn_post': gain(ks[4], (DEPTH, D_MODEL)),
        'w_in': nrm(ks[5], (DEPTH, D_MODEL, D_IN), D_MODEL ** -0.5),
        'conf_dw_w': nrm(ks[6], (DEPTH, CONF_KERNEL, GROUP_WIDTH), CONF_KERNEL ** -0.5),
        'conf_dw_b': nrm(ks[7], (DEPTH, GROUP_WIDTH), 0.02),
        'conf_ln_g': gain(ks[8], (DEPTH, GROUP_WIDTH)),
        'conf_ln_b': nrm(ks[9], (DEPTH, GROUP_WIDTH), 0.02),
        'ret_norm_g': gain(ks[10], (DEPTH, GROUP_WIDTH)),
        'sc_conv_w': nrm(ks[11], (DEPTH, SHORT_KERNEL, GROUP_WIDTH), SHORT_KERNEL ** -0.5),
        'w_out': nrm(ks[12], (DEPTH, D_MIX, D_MODEL), D_MIX ** -0.5),
        'ffn_up': nrm(ks[13], (DEPTH, D_MODEL, 2 * D_FF), D_MODEL ** -0.5),
        'ffn_conv_w': nrm(ks[14], (DEPTH, FFN_KERNEL, 2 * D_FF), FFN_KERNEL ** -0.5),
        'ffn_down': nrm(ks[15], (DEPTH, D_FF, D_MODEL), D_FF ** -0.5),
    }


def reference(x, norm_mix_pre, norm_mix_post, norm_ffn_pre, norm_ffn_post, w_in,
              conf_dw_w, conf_dw_b, conf_ln_g, conf_ln_b, ret_norm_g, sc_conv_w,
              w_out, ffn_up, ffn_conv_w, ffn_down):
    for l in range(DEPTH):
        h = rms_norm(x, norm_mix_pre[l])
        proj = h @ w_in[l]
        (c_a, c_gate, sb_q, sb_k, sb_v, r_q, r_k, r_v, r_g,
         sc_b, sc_c, sc_h) = jnp.split(proj, N_IN_SLICES, axis=-1)
        y_conf = conformer_conv(c_a, c_gate, conf_dw_w[l], conf_dw_b[l], conf_ln_g[l], conf_ln_b[l])
        y_sb = merge_heads(stick_breaking_attention(split_heads(sb_q), split_heads(sb_k), split_heads(sb_v)))
        y_ret = retention_mixer(r_q, r_k, r_v, r_g, ret_norm_g[l])
        y_sc = short_gated_conv(sc_b, sc_c, sc_h, sc_conv_w[l])
        mix = jnp.concatenate([y_conf, y_sb, y_ret, y_sc], axis=-1) @ w_out[l]
        x = x + rms_norm(mix, norm_mix_post[l])
        f = conv_ffn(rms_norm(x, norm_ffn_pre[l]), ffn_up[l], ffn_conv_w[l], ffn_down[l])
        x = x + rms_norm(f, norm_ffn_post[l])
    return x
```

```python
import concourse.bass as bass
import concourse.mybir as mybir

F32 = mybir.dt.float32
BF16 = mybir.dt.bfloat16
AF = mybir.ActivationFunctionType
ALU = mybir.AluOpType
AX = mybir.AxisListType

ENGS = ("pe", "act", "dve", "pool", "sp")


class Buf:
    __slots__ = ("name", "w", "r")

    def __init__(self, name):
        self.name = name
        self.w = []
        self.r = []


class V:
    __slots__ = ("ap", "bufs")

    def __init__(self, ap, bufs):
        self.ap = ap
        self.bufs = tuple(bufs)

    def __getitem__(self, idx):
        return V(self.ap[idx], self.bufs)

    def bc(self, shape):
        return V(self.ap.to_broadcast(shape), self.bufs)

    def re(self, pattern, **kw):
        return V(self.ap.rearrange(pattern, **kw), self.bufs)

    def wb(self, *bufs):
        return V(self.ap, bufs)


class Sched:
    def __init__(self, nc, n_dma_sems=20, same_engine_sync=True):
        self.nc = nc
        self.streams = {e: [] for e in ENGS}
        self.cnt = {e: 0 for e in ENGS}
        self.waited = {e: {} for e in ENGS}
        self.same_engine_sync = same_engine_sync
        self.n_dma_sems = n_dma_sems
        self.dma_val = [0] * n_dma_sems
        self.dma_next = 0
        self.n_ops = 0
        self.n_waits = 0
        self.n_sw = 0

    def _need(self, eng, waits, ev):
        key, val = ev
        if key == eng and (eng == "pe" or not self.same_engine_sync):
            return
        if self.waited[eng].get(key, 0) >= val:
            return
        if waits.get(key, 0) < val:
            waits[key] = val

    def _deps(self, eng, outs, ins, waits):
        for v in ins:
            for b in v.bufs:
                for ev in b.w:
                    self._need(eng, waits, ev)
        for v in outs:
            for b in v.bufs:
                for ev in b.w:
                    self._need(eng, waits, ev)
                for ev in b.r:
                    self._need(eng, waits, ev)

    def _commit(self, eng, waits, outs, ins, ev):
        for k, val in waits.items():
            self.waited[eng][k] = max(self.waited[eng].get(k, 0), val)
        for v in ins:
            for b in v.bufs:
                b.r.append(ev)
        for v in outs:
            for b in v.bufs:
                b.w = [ev]
                b.r = []

    def op(self, eng, fn, outs, ins):
        waits = {}
        self._deps(eng, outs, ins, waits)
        self.cnt[eng] += 1
        ev = (eng, self.cnt[eng])
        self._commit(eng, waits, outs, ins, ev)
        self.streams[eng].append((waits, fn, (eng, 1)))
        self.n_ops += 1
        self.n_waits += len(waits)

    def dma(self, q, out, in_, **kw):
        waits = {}
        self._deps(q, [out], [in_], waits)
        if q == "pool":
            key = ("sw", self.n_sw)
            self.n_sw += 1
            ev = (key, 16)
        else:
            k = self.dma_next
            self.dma_next = (k + 1) % self.n_dma_sems
            key = ("dma", k)
            if self.dma_val[k] > 0:
                self._need(q, waits, (key, self.dma_val[k]))
            self.dma_val[k] += 16
            ev = (key, self.dma_val[k])
        self._commit(q, waits, [out], [in_], ev)
        o_ap, i_ap = out.ap, in_.ap
        self.streams[q].append(
            (waits, lambda e: e.dma_start(out=o_ap, in_=i_ap, **kw), (key, 16)))
        self.n_ops += 1
        self.n_waits += len(waits)
        return ev

    def barrier(self):
        evs = [(e, self.cnt[e]) for e in ENGS if self.cnt[e] > 0]
        evs += [(("dma", k), v) for k, v in enumerate(self.dma_val) if v > 0]
        evs += [(("sw", k), 16) for k in range(self.n_sw)]
        for e in ENGS:
            waits = {}
            for ev in evs:
                if ev[0] == e:
                    continue
                self._need(e, waits, ev)
            if waits:
                for k, val in waits.items():
                    self.waited[e][k] = max(self.waited[e].get(k, 0), val)
                self.streams[e].append((waits, None, None))

    def emit(self, final_events=()):
        nc = self.nc
        from contextlib import ExitStack
        with ExitStack() as es:
            sems = {}
            for e in ENGS:
                sems[e] = es.enter_context(nc.semaphore("s_" + e))
            for k in range(self.n_dma_sems):
                sems[("dma", k)] = es.enter_context(nc.semaphore("s_dma%d" % k))
            for k in range(self.n_sw):
                sems[("sw", k)] = es.enter_context(nc.semaphore("s_sw%d" % k))
            block = es.enter_context(nc.Block())
            streams = self.streams

            def run(engname, eng):
                for waits, fn, inc in streams[engname]:
                    for k, val in waits.items():
                        eng.wait_ge(sems[k], val)
                    if fn is None:
                        continue
                    ins = fn(eng)
                    ins.then_inc(sems[inc[0]], inc[1])

            @block.tensor
            def _(e):
                run("pe", e)

            @block.scalar
            def _(e):
                run("act", e)

            @block.vector
            def _(e):
                run("dve", e)

            @block.gpsimd
            def _(e):
                run("pool", e)

            @block.sync
            def _(e):
                run("sp", e)
                for k in range(self.n_dma_sems):
                    if self.dma_val[k] > 0:
                        e.wait_ge(sems[("dma", k)], self.dma_val[k])
                for k in range(self.n_sw):
                    e.wait_ge(sems[("sw", k)], 16)

    def matmul(self, out, lhsT, rhs, start=True, stop=True, **kw):
        o, l, r = out.ap, lhsT.ap, rhs.ap
        self.op("pe", lambda e: e.matmul(o, l, r, start=start, stop=stop, **kw),
                [out], [lhsT, rhs])

    def transpose(self, out, in_, ident):
        o, i, d = out.ap, in_.ap, ident.ap
        self.op("pe", lambda e: e.transpose(o, i, d), [out], [in_, ident])

    def act(self, out, in_, func, bias=None, scale=None, accum_out=None):
        ins = [in_]
        outs = [out]
        kw = {}
        if bias is not None:
            if isinstance(bias, V):
                ins.append(bias)
                kw["bias"] = bias.ap
            else:
                kw["bias"] = float(bias)
        if scale is not None:
            if isinstance(scale, V):
                ins.append(scale)
                kw["scale"] = scale.ap
            else:
                kw["scale"] = float(scale)
        if accum_out is not None:
            outs.append(accum_out)
            kw["accum_out"] = accum_out.ap
        o, i = out.ap, in_.ap
        self.op("act", lambda e: e.activation(out=o, in_=i, func=func, **kw), outs, ins)

    def tt(self, eng, out, in0, in1, op):
        o, a, b = out.ap, in0.ap, in1.ap
        self.op(eng, lambda e: e.tensor_tensor(out=o, in0=a, in1=b, op=op), [out], [in0, in1])

    def ts(self, eng, out, in0, s1, op0, s2=None, op1=None):
        ins = [in0]
        a1 = s1.ap if isinstance(s1, V) else float(s1)
        if isinstance(s1, V):
            ins.append(s1)
        a2 = None
        if s2 is not None:
            a2 = s2.ap if isinstance(s2, V) else float(s2)
            if isinstance(s2, V):
                ins.append(s2)
        o, a = out.ap, in0.ap
        if op1 is None:
            self.op(eng, lambda e: e.tensor_scalar(out=o, in0=a, scalar1=a1, scalar2=a2, op0=op0),
                    [out], ins)
        else:
            self.op(eng, lambda e: e.tensor_scalar(out=o, in0=a, scalar1=a1, scalar2=a2, op0=op0, op1=op1),
                    [out], ins)

    def stt(self, out, in0, scalar, in1, op0, op1):
        ins = [in0, in1]
        sc = scalar.ap if isinstance(scalar, V) else float(scalar)
        if isinstance(scalar, V):
            ins.append(scalar)
        o, a, b = out.ap, in0.ap, in1.ap
        self.op("dve", lambda e: e.scalar_tensor_tensor(out=o, in0=a, scalar=sc, in1=b, op0=op0, op1=op1),
                [out], ins)

    def copy(self, eng, out, in_):
        o, i = out.ap, in_.ap
        if eng == "act":
            self.op("act", lambda e: e.copy(out=o, in_=i), [out], [in_])
        else:
            self.op(eng, lambda e: e.tensor_copy(out=o, in_=i), [out], [in_])

    def reduce(self, eng, out, in_, op, axis=AX.X):
        o, i = out.ap, in_.ap
        self.op(eng, lambda e: e.tensor_reduce(out=o, in_=i, axis=axis, op=op), [out], [in_])

    def memset(self, eng, out, val):
        o = out.ap
        self.op(eng, lambda e: e.memset(o, val), [out], [])

import numpy as np
from contextlib import ExitStack
from concourse.bass_utils import run_bass_kernel_spmd

DM = 1024
DIN = 3072
DFF = 2816
NFT = 22
TT = 256
EPS = 1e-6
NLP = 62 + 2 + 2 + 2 + 6 + 132
LP_DWW, LP_DWB, LP_LNG, LP_LNB, LP_SCW, LP_FFW = 0, 62, 64, 66, 68, 74
NROW = 4 * 1024 + 256
C_ID, C_M1, C_CAUS, C_MASK, C_QKD, C_CD = 0, 128, 256, 384, 384 + 512, 384 + 512 + 8
NCON = C_CD + 2


def host_consts(T):
    c = np.zeros((128, NCON), np.float32)
    j = np.arange(128)
    c[:, C_ID:C_ID + 128] = np.eye(128)
    c[:, C_M1:C_M1 + 128] = -(j[:, None] >= j[None, :]).astype(np.float32)
    c[:, C_CAUS:C_CAUS + 128] = (j[None, :] >= j[:, None]).astype(np.float32)
    t = np.arange(256)
    for r in range(2):
        c[:, C_MASK + r * 256:C_MASK + (r + 1) * 256] = ((128 * r + j)[:, None] < t[None, :]).astype(np.float32)
    lg = np.log1p(-np.exp2(-5.0 - np.arange(4, dtype=np.float64)))
    for h in range(4):
        c[:, C_QKD + h] = np.exp((j + 1.0) * lg[h])
        c[:, C_QKD + 4 + h] = np.exp(-(j + 1.0) * lg[h]) * 0.125
    for idx in range(2):
        for half in range(2):
            c[half * 64:(half + 1) * 64, C_CD + idx] = np.exp(128.0 * lg[2 * idx + half])
    pos = np.arange(T, dtype=np.float32)
    inv = (10000.0 ** (-np.arange(0, 64, 2, dtype=np.float32) / 64)).astype(np.float32)
    ang = (pos[:, None] * inv[None, :]).astype(np.float32)
    cs = np.concatenate([np.cos(ang), np.sin(ang)], -1).astype(np.float32)
    cs = np.ascontiguousarray(cs.reshape(T // 128, 128, 64).transpose(1, 0, 2))
    return c, cs


def build(T, NL, dbg=False, same_engine_sync=True):
    import os
    KSTOP = int(os.environ.get("KSTOP", "99"))
    KNOFFN = int(os.environ.get("KNOFFN", "0"))
    NT = T // TT
    NB = T // 128
    nc = bass.Bass("TRN2", target_bir_lowering=False)
    dram = lambda n, s, k="ExternalInput", dt=F32: nc.dram_tensor(n, list(s), dt, kind=k).ap()
    x_d = dram("x", [T, DM])
    win_d = dram("w_in", [NL, DM, DIN])
    wout_d = dram("w_out", [NL, DM, DM])
    wup_d = dram("ffn_up", [NL, DM, 2 * DFF])
    wdn_d = dram("ffn_down", [NL, DFF, DM])
    lp_d = dram("lp", [NL, 128, NLP])
    rowp_d = dram("rowp", [NL, 1, NROW])
    con_d = dram("consts", [128, NCON])
    cs_d = dram("cs", [128, NB, 64])
    y_d = dram("y", [T, DM], "ExternalOutput")
    xa_d = dram("xa", [T, DM], "Internal")
    xb_d = dram("xb", [T, DM], "Internal")
    if dbg:
        dmix_d = dram("dbg_mix", [NT, 128, 8, TT], "ExternalOutput", BF16)
        dxa_d = dram("dbg_xa", [T, DM], "ExternalOutput")

    S = Sched(nc, same_engine_sync=same_engine_sync)
    xbufs = {}

    def xview(ap, name, tt, s):
        bl = xbufs.setdefault(name, [Buf("%s%d" % (name, i)) for i in range(NT)])
        r0 = tt * TT + s * 128
        return V(ap[r0:r0 + 128, :], [bl[tt]])

    with ExitStack() as top:
        uid = [0]

        def sbt(es, name, shape, dt):
            uid[0] += 1
            return es.enter_context(nc.sbuf_tensor("%s_u%d" % (name, uid[0]), list(shape), dt))

        def alloc(es, name, shape, dt):
            t = sbt(es, name, shape, dt)
            return V(t[:], [Buf(name)])

        banks = []
        for i in range(8):
            t = top.enter_context(nc.psum_tensor("bank%d" % i, [128, 512], F32))
            banks.append(V(t[:], [Buf("bank%d" % i)]))

        class Rot:
            def __init__(self, items):
                self.items = items
                self.i = 0

            def get(self):
                v = self.items[self.i % len(self.items)]
                self.i += 1
                return v

        def bfview(bank):
            return V(bank.ap.bitcast(BF16), bank.bufs)

        con = alloc(top, "con", [128, NCON], F32)
        S.dma("sp", con, V(con_d, [Buf("con_d")]))
        identb = alloc(top, "identb", [128, 128], BF16)
        negm1 = alloc(top, "negm1", [128, 128], BF16)
        negones = alloc(top, "negones", [128, 128], BF16)
        onesm = alloc(top, "onesm", [128, 128], BF16)
        S.copy("dve", identb, con[:, C_ID:C_ID + 128])
        S.copy("dve", negm1, con[:, C_M1:C_M1 + 128])
        S.memset("dve", negones, -1.0)
        S.memset("dve", onesm, 1.0 / 256)
        identf = con[:, C_ID:C_ID + 128]
        caus = con[:, C_CAUS:C_CAUS + 128]
        maskd = [con[:, C_MASK + r * 256:C_MASK + (r + 1) * 256] for r in range(2)]
        qkd = con[:, C_QKD:C_QKD + 8]
        cdt = con[:, C_CD:C_CD + 2]
        cs_dv = V(cs_d, [Buf("cs_d")])

        def norm_transpose(tt, srcname, src_ap, xs, ssv, rs, junk, hbs, hT, gpre, gen):
            for s in range(2):
                S.dma("sp", xs[s], xview(src_ap, srcname, tt, s))
                S.act(hbs[s], xs[s], AF.Square, accum_out=ssv[s])
                S.act(rs[s], ssv[s], AF.Ln, scale=1.0 / DM, bias=EPS)
                S.act(rs[s], rs[s], AF.Exp, scale=-0.5)
                S.stt(hbs[s], xs[s], rs[s], gpre, ALU.mult, ALU.mult)
                pb = bfview(gen.get())
                for kc in range(8):
                    S.transpose(pb[:, kc * 128:(kc + 1) * 128], hbs[s][:, kc * 128:(kc + 1) * 128], identb)
                S.copy("act" if s == 0 else "dve", hT[:, :, s * 128:(s + 1) * 128],
                       pb.re("p (a b) -> p a b", b=128))

        def post_residual(tt, s, bk, srcname, src_ap, dstname, dst_ap, xs, junk, ssp, ssum, rs2, gpost, tmp):
            for g in range(2):
                S.act(junk[:, 0:512], bk[g], AF.Square, accum_out=ssp[:, g:g + 1])
            S.tt("dve", ssum, ssp[:, 0:1], ssp[:, 1:2], ALU.add)
            S.act(rs2, ssum, AF.Ln, scale=1.0 / DM, bias=EPS)
            S.act(rs2, rs2, AF.Exp, scale=-0.5)
            S.dma("sp", xs[s], xview(src_ap, srcname, tt, s))
            for g in range(2):
                S.stt(tmp[:, g * 512:(g + 1) * 512], bk[g], rs2, gpost[:, g * 512:(g + 1) * 512],
                      ALU.mult, ALU.mult)
            S.tt("pool", tmp, tmp, xs[s], ALU.add)
            S.dma("sp", xview(dst_ap, dstname, tt, s), tmp)

        def mixer_phase(l, srcname, src_ap, dstname, dst_ap):
            with ExitStack() as es:
                A_ = lambda n, s, d: alloc(es, n, s, d)
                win_t = sbt(es, "win", [128, 8, DIN], BF16)
                wb_ = [Buf("winA"), Buf("winB")]
                win = [V(win_t[:][:, kc, :], [wb_[kc // 4]]) for kc in range(8)]
                wout_t = sbt(es, "wout", [128, 8, DM], BF16)
                wob_ = Buf("woutA")
                wout = [V(wout_t[:][:, kc, :], [wob_]) for kc in range(8)]
                lp = A_("lp", [128, NLP], F32)
                gpre = A_("gpre", [128, DM], F32)
                gpost = A_("gpost", [128, DM], F32)
                retg = A_("retg", [128, 256], F32)
                diag = A_("diag", [128, 62, 128], BF16)
                rowv = V(rowp_d[l], [Buf("rowp_d")])
                S.dma("sp", lp, V(lp_d[l], [Buf("lp_d")]))
                S.dma("sp", gpre, rowv[:, 0:1024].bc([128, 1024]))
                S.dma("sp", gpost, rowv[:, 1024:2048].bc([128, 1024]))
                S.dma("sp", retg, rowv[:, 4096:4352].bc([128, 256]))
                wv = V(win_d[l].rearrange("(kc p) n -> p kc n", p=128), [Buf("win_d")])
                for g in range(2):
                    S.dma("pool", V(win_t[:][:, 4 * g:4 * g + 4, :], [wb_[g]]), wv[:, 4 * g:4 * g + 4, :])
                wv = V(wout_d[l].rearrange("(kc p) n -> p kc n", p=128), [Buf("wout_d")])
                S.dma("pool", V(wout_t[:], [wob_]), wv)
                for i in range(62):
                    S.ts("dve" if i % 2 else "pool", diag[:, i, :], identf, lp[:, LP_DWW + i:LP_DWW + i + 1], ALU.mult)

                xs = [A_("xs%d" % s, [128, DM], F32) for s in range(2)]
                hbs = [A_("hbs%d" % s, [128, DM], BF16) for s in range(2)]
                ssv = [A_("ssv%d" % s, [128, 1], F32) for s in range(2)]
                rs = [A_("rs%d" % s, [128, 1], F32) for s in range(2)]
                junk = A_("junk", [128, 512], BF16)
                hT = A_("hT", [128, 8, TT], BF16)
                cst = A_("cst", [128, 2, 64], F32)
                kT_t = sbt(es, "kT", [128, 2, T], BF16)
                kTb = [Buf("kT%d" % i) for i in range(NT)]
                vc_t = sbt(es, "vc", [128, NB, 256], BF16)
                vcb = [Buf("vc%d" % i) for i in range(NT)]
                qTs = [A_("qT%d" % i, [128, 2, TT], BF16) for i in range(2)]
                xsB1 = A_("xsB", [128, DM], F32)
                xsB = [xsB1, xsB1]
                Ub = [[A_("Ub%d%d" % (ct, p), [128, 30 + TT], BF16) for p in range(2)] for ct in range(2)]
                sgm = A_("sgm", [128, TT], F32)
                cvb = A_("cvb", [128, 2, TT], F32)
                cvh = A_("cvh", [128, 2, TT], BF16)
                sqb = A_("sqb", [128, 2, TT], BF16)
                mean_sb = A_("mean_sb", [128, TT], F32)
                var_sb = A_("var_sb", [128, TT], F32)
                ytmp = A_("ytmp", [128, TT], F32)
                Csb = A_("Csb", [128, TT], F32)
                Bsb = A_("Bsb", [128, 2, TT], BF16)
                Pb = [A_("Pb%d" % ct, [128, 2 + TT], F32) for ct in range(2)]
                tcv = A_("tcv", [128, TT], F32)
                r32 = A_("r32", [128, 512], F32)
                rot = A_("rot", [128, 512], F32)
                rta = A_("rta", [128, 256], F32)
                rtb = A_("rtb", [128, 256], F32)
                rtc = A_("rtc", [128, 256], F32)
                rtd = A_("rtd", [128, 256], F32)
                rqkb = A_("rqkb", [128, 512], BF16)
                rqkT = A_("rqkT", [128, 4, 128], BF16)
                rvb = A_("rvb", [128, 256], BF16)
                sgate = A_("sgate", [128, 256], F32)
                innD = A_("innD", [128, 512], BF16)
                osb = A_("osb", [128, 256], F32)
                osq = A_("osq", [128, 256], F32)
                st4 = [A_("st4_%d" % i, [128, 4], F32) for i in range(4)]
                yn = A_("yn", [128, 256], F32)
                ybf = A_("ybf", [128, 256], BF16)
                S32 = A_("S32", [128, 2, 64], F32)
                Sbf = A_("Sbf", [128, 2, 64], BF16)
                e_ = [[A_("e%d_%d" % (i, h), [128, TT], F32) for h in range(2)] for i in range(3)]
                sp_ = [[A_("sp%d_%d" % (i, h), [128, TT], BF16) for h in range(2)] for i in range(2)]
                Aacc = [A_("Aacc%d" % h, [128, TT], BF16) for h in range(2)]
                E2 = [A_("E2_%d" % h, [128, TT], F32) for h in range(2)]
                attn = [[A_("attn%d_%d" % (i, h), [128, TT], BF16) for h in range(2)] for i in range(2)]
                mixTs = [A_("mixT%d" % i, [128, 8, TT], BF16) for i in range(2)]
                ssp = A_("ssp", [128, 2], F32)
                ssum = A_("ssum", [128, 1], F32)
                rs2 = A_("rs2", [128, 1], F32)
                tmp1 = A_("tmp", [128, DM], F32)
                tmp = [tmp1, tmp1]

                gen = Rot(banks[0:2])
                zsl = [banks[2 + hh][:, 0:TT] for hh in range(2)]
                tsl = [banks[4 + hh][:, 0:TT] for hh in range(2)]
                posl = [banks[6 + hh][hh * 64:hh * 64 + 64, :] for hh in range(2)]
                for ct in range(2):
                    S.memset("pool", Ub[ct][1][:, TT:TT + 30], 0.0)
                    S.memset("pool", Pb[ct][:, TT:TT + 2], 0.0)
                S.memset("pool", S32, 0.0)
                S.memset("pool", Sbf, 0.0)

                def proj_feat(dst, col0):
                    for kc in range(8):
                        S.matmul(dst, win[kc][:, col0:col0 + 128], hT[:, kc, :], start=(kc == 0), stop=(kc == 7))

                def stage_A(tt):
                    mT = mixTs[tt % 2]
                    qTt = qTs[tt % 2]
                    t0 = tt * TT
                    par = tt % 2
                    kTv = V(kT_t[:][:, :, t0:t0 + TT], [kTb[tt]])
                    norm_transpose(tt, srcname, src_ap, xs, ssv, rs, junk, hbs, hT, gpre, gen)
                    S.dma("sp", cst, cs_dv[:, 2 * tt:2 * tt + 2, :])
                    yield
                    for ct in range(2):
                        bk = gen.get()
                        proj_feat(bk[:, 0:TT], ct * 128)
                        proj_feat(bk[:, TT:2 * TT], 256 + ct * 128)
                        S.act(sgm, bk[:, TT:2 * TT], AF.Sigmoid)
                        S.copy("pool", Ub[ct][par][:, 0:30], Ub[ct][1 - par][:, TT:TT + 30])
                        S.tt("dve", Ub[ct][par][:, 30:30 + TT], bk[:, 0:TT], sgm, ALU.mult)
                        yield
                    yield
                    for ct in range(2):
                        bk = gen.get()
                        proj_feat(bk[:, 0:TT], 512 + ct * 128)
                        proj_feat(bk[:, TT:2 * TT], 768 + ct * 128)
                        S.ts("dve", qTt[:, ct, :], bk[:, 0:TT], 0.125, ALU.mult)
                        S.copy("dve", kTv[:, ct, :], bk[:, TT:2 * TT])
                        yield
                    yield
                    for ct in range(2):
                        bk = gen.get()
                        bk2 = gen.get()
                        proj_feat(bk[:, 0:TT], 2304 + ct * 128)
                        proj_feat(bk[:, TT:2 * TT], 2560 + ct * 128)
                        proj_feat(bk2[:, 0:TT], 2816 + ct * 128)
                        S.copy("act", Bsb[:, ct, :], bk[:, 0:TT])
                        S.copy("act", Csb, bk[:, TT:2 * TT])
                        S.copy("pool", Pb[ct][:, 0:2], Pb[ct][:, TT:TT + 2])
                        S.tt("dve", Pb[ct][:, 2:2 + TT], bk2[:, 0:TT], Csb, ALU.mult)
                        yield
                    yield
                    for ct in range(2):
                        bk = gen.get()
                        for k in range(31):
                            S.matmul(bk[:, 0:TT], diag[:, ct * 31 + k, :], Ub[ct][par][:, k:k + TT],
                                     start=(k == 0), stop=(k == 30))
                        S.ts("dve", cvb[:, ct, :], bk[:, 0:TT], lp[:, LP_DWB + ct:LP_DWB + ct + 1], ALU.add)
                        S.act(sqb[:, ct, :], cvb[:, ct, :], AF.Square)
                        S.copy("dve", cvh[:, ct, :], cvb[:, ct, :])
                        yield
                    bk = gen.get()
                    for ct in range(2):
                        S.matmul(bk[:, 0:TT], onesm, cvh[:, ct, :], start=(ct == 0), stop=(ct == 1))
                    for ct in range(2):
                        S.matmul(bk[:, TT:2 * TT], onesm, sqb[:, ct, :], start=(ct == 0), stop=(ct == 1))
                    S.copy("act", mean_sb, bk[:, 0:TT])
                    S.tt("dve", var_sb, mean_sb, mean_sb, ALU.mult)
                    S.tt("dve", var_sb, bk[:, TT:2 * TT], var_sb, ALU.subtract)
                    S.act(var_sb, var_sb, AF.Ln, bias=EPS)
                    S.act(var_sb, var_sb, AF.Exp, scale=-0.5)
                    for ct in range(2):
                        S.tt("dve", ytmp, cvb[:, ct, :], mean_sb, ALU.subtract)
                        S.tt("dve", ytmp, ytmp, var_sb, ALU.mult)
                        S.act(mT[:, ct, :], ytmp, AF.Silu, scale=lp[:, LP_LNG + ct:LP_LNG + ct + 1],
                              bias=lp[:, LP_LNB + ct:LP_LNB + ct + 1])
                    yield
                    for ct in range(2):
                        w = lambda k: lp[:, LP_SCW + ct * 3 + k:LP_SCW + ct * 3 + k + 1]
                        S.ts("dve", tcv, Pb[ct][:, 2:2 + TT], w(2), ALU.mult)
                        S.stt(tcv, Pb[ct][:, 1:1 + TT], w(1), tcv, ALU.mult, ALU.add)
                        S.stt(tcv, Pb[ct][:, 0:TT], w(0), tcv, ALU.mult, ALU.add)
                        S.tt("dve", mT[:, 6 + ct, :], tcv, Bsb[:, ct, :], ALU.mult)
                    yield
                    for s in range(2):
                        cb = tt * 2 + s
                        tok = slice(s * 128, (s + 1) * 128)
                        bA = gen.get()
                        for kc in range(8):
                            S.matmul(bA, hT[:, kc, tok], win[kc][:, 1280:1792], start=(kc == 0), stop=(kc == 7))
                        S.copy("act", r32, bA)
                        bB = gen.get()
                        for kc in range(8):
                            S.matmul(bB, hT[:, kc, tok], win[kc][:, 1792:2304], start=(kc == 0), stop=(kc == 7))
                        S.copy("act", rvb, bB[:, 0:256])
                        S.act(sgate, bB[:, 256:512], AF.Silu)
                        bC = gen.get()
                        for kc in range(8):
                            S.matmul(bC[:, 0:256], hT[:, kc, tok], win[kc][:, 1024:1280], start=(kc == 0), stop=(kc == 7))
                        S.copy("act", V(vc_t[:][:, cb, :], [vcb[tt]]), bC[:, 0:256])
                        yield
                        xv = r32.re("p (g two f) -> p g two f", two=2, f=32)
                        ov = rot.re("p (g two f) -> p g two f", two=2, f=32)
                        x1, x2 = xv[:, :, 0, :], xv[:, :, 1, :]
                        cosb = cst[:, s, 0:32].re("p (o f) -> p o f", o=1).bc([128, 8, 32])
                        sinb = cst[:, s, 32:64].re("p (o f) -> p o f", o=1).bc([128, 8, 32])
                        v3 = lambda t: t.re("p (g f) -> p g f", f=32)
                        S.tt("dve", v3(rta), x1, cosb, ALU.mult)
                        S.tt("dve", v3(rtb), x2, sinb, ALU.mult)
                        S.tt("dve", ov[:, :, 0, :], v3(rta), v3(rtb), ALU.subtract)
                        S.tt("pool", v3(rtc), x2, cosb, ALU.mult)
                        S.tt("pool", v3(rtd), x1, sinb, ALU.mult)
                        S.tt("pool", ov[:, :, 1, :], v3(rtc), v3(rtd), ALU.add)
                        S.tt("dve", rqkb.re("p (g f) -> p g f", f=64), rot.re("p (g f) -> p g f", f=64),
                             qkd.re("p (g o) -> p g o", o=1).bc([128, 8, 64]), ALU.mult)
                        pb = bfview(gen.get())
                        for j in range(4):
                            S.transpose(pb[:, j * 128:(j + 1) * 128], rqkb[:, j * 128:(j + 1) * 128], identb)
                        S.copy("act", rqkT, pb[:, 0:512].re("p (a b) -> p a b", b=128))
                        yield
                        bI = [gen.get(), gen.get()]
                        for h in range(4):
                            pr = slice((h % 2) * 64, (h % 2) * 64 + 64)
                            S.matmul(bI[h % 2][:, (h // 2) * 128:(h // 2) * 128 + 128], rqkT[pr, 2 + h // 2, :],
                                     rqkT[pr, h // 2, :])
                        for par2 in range(2):
                            S.tt("dve", innD[:, par2 * 256:(par2 + 1) * 256].re("p (h q) -> p h q", q=128),
                                 bI[par2][:, 0:256].re("p (h q) -> p h q", q=128),
                                 caus.re("p (o q) -> p o q", o=1).bc([128, 2, 128]), ALU.mult)
                        bO = [gen.get(), gen.get()]
                        for h in range(4):
                            pr = slice((h % 2) * 64, (h % 2) * 64 + 64)
                            j = (h % 2) * 2 + h // 2
                            dst = bO[h % 2][:, (h // 2) * 64:(h // 2) * 64 + 64]
                            S.matmul(dst, innD[:, j * 128:(j + 1) * 128],
                                     rvb[:, h * 64:(h + 1) * 64], start=True, stop=False)
                            S.matmul(dst, rqkT[pr, h // 2, :], Sbf[pr, h // 2, :],
                                     start=False, stop=True)
                        for par2 in range(2):
                            S.copy("act", osb.re("p (a b e) -> p a b e", b=2, e=64)[:, :, par2, :],
                                   bO[par2][:, 0:128].re("p (a e) -> p a e", e=64))
                        bS = gen.get()
                        for h in range(4):
                            pr = slice((h % 2) * 64, (h % 2) * 64 + 64)
                            S.matmul(bS[pr, (h // 2) * 64:(h // 2) * 64 + 64], rqkb[:, 256 + h * 64:256 + (h + 1) * 64],
                                     rvb[:, h * 64:(h + 1) * 64])
                        S.tt("dve", S32, S32, bS[:, 0:128].re("p (a e) -> p a e", e=64), ALU.add)
                        S.tt("dve", S32, S32, cdt.re("p (a o) -> p a o", o=1).bc([128, 2, 64]), ALU.mult)
                        S.copy("dve", Sbf, S32)
                        yield
                        S.act(osq, osb, AF.Square)
                        o3 = lambda t: t.re("p (h e) -> p h e", e=64)
                        S.reduce("dve", st4[0], o3(osb), ALU.add)
                        S.reduce("dve", st4[1], o3(osq), ALU.add)
                        S.ts("dve", st4[0], st4[0], 1.0 / 64, ALU.mult)
                        S.tt("dve", st4[2], st4[0], st4[0], ALU.mult)
                        S.stt(st4[3], st4[1], 1.0 / 64, st4[2], ALU.mult, ALU.subtract)
                        S.act(st4[3], st4[3], AF.Ln, bias=EPS)
                        S.act(st4[3], st4[3], AF.Exp, scale=-0.5)
                        b4 = lambda t: t.re("p (h o) -> p h o", o=1).bc([128, 4, 64])
                        S.tt("dve", o3(yn), o3(osb), b4(st4[0]), ALU.subtract)
                        S.tt("dve", o3(yn), o3(yn), b4(st4[3]), ALU.mult)
                        S.tt("pool", yn, yn, retg, ALU.mult)
                        S.tt("pool", ybf, yn, sgate, ALU.mult)
                        pb = bfview(gen.get())
                        for j in range(2):
                            S.transpose(pb[:, j * 128:(j + 1) * 128], ybf[:, j * 128:(j + 1) * 128], identb)
                        S.copy("act", mT[:, 4:6, tok], pb[:, 0:256].re("p (a b) -> p a b", b=128))
                        yield

                def stage_B(tt):
                    mT = mixTs[tt % 2]
                    qTt = qTs[tt % 2]
                    nkb = 2 * tt + 2
                    kbs = list(range(nkb - 1, -1, -1))
                    for hp in range(2):
                        def st1a(n):
                            kb = kbs[n]
                            for hh in range(2):
                                pr = slice(hh * 64, hh * 64 + 64)
                                kblk = V(kT_t[:][pr, hp, kb * 128:(kb + 1) * 128], [kTb[kb // 2]])
                                S.matmul(zsl[hh], kblk, qTt[pr, hp, :])

                        def st1b(n):
                            for hh in range(2):
                                S.act(e_[n % 3][hh], zsl[hh], AF.Exp)

                        def st2(n):
                            kb = kbs[n]
                            r = kb - 2 * tt
                            first, last = (n == 0), (kb == 0)
                            for hh in range(2):
                                e, sp = e_[n % 3][hh], sp_[n % 2][hh]
                                S.act(sp, e, AF.Ln, bias=1.0)
                                if r >= 0:
                                    S.tt("pool", sp, sp, maskd[r], ALU.mult)
                                    S.tt("pool", e, e, maskd[r], ALU.mult)
                            for hh in range(2):
                                S.matmul(tsl[hh], negm1, sp_[n % 2][hh], start=True, stop=first)
                                if not first:
                                    S.matmul(tsl[hh], negones, Aacc[hh], start=False, stop=True)
                            if not last:
                                for hh in range(2):
                                    if first:
                                        S.copy("pool", Aacc[hh], sp_[n % 2][hh])
                                    else:
                                        S.tt("pool", Aacc[hh], Aacc[hh], sp_[n % 2][hh], ALU.add)

                        def st3(n):
                            kb = kbs[n]
                            first, last = (n == 0), (kb == 0)
                            for hh in range(2):
                                S.act(E2[hh], tsl[hh], AF.Exp)
                                S.tt("dve", attn[n % 2][hh], e_[n % 3][hh], E2[hh], ALU.mult)
                            for hh in range(2):
                                h = 2 * hp + hh
                                vblk = V(vc_t[:][:, kb, h * 64:(h + 1) * 64], [vcb[kb // 2]])
                                S.matmul(posl[hh][:, hp * TT:(hp + 1) * TT], vblk, attn[n % 2][hh],
                                         start=first, stop=last)

                        for itn in range(nkb + 2):
                            if itn < nkb:
                                st1a(itn)
                            if itn - 2 >= 0:
                                st3(itn - 2)
                            if 0 <= itn - 1 < nkb:
                                st2(itn - 1)
                            if itn < nkb:
                                st1b(itn)
                            yield
                        for hh in range(2):
                            pr = slice(hh * 64, hh * 64 + 64)
                            S.copy("act", mT[pr, 2 + hp, :], posl[hh][:, hp * TT:(hp + 1) * TT])
                    yield
                    if dbg:
                        S.dma("sp", V(dmix_d[tt], [Buf("dmix%d" % tt)]), mT)
                    for s in range(2):
                        tok = slice(s * 128, (s + 1) * 128)
                        bk = [gen.get(), gen.get()]
                        for g in range(2):
                            for kc in range(8):
                                S.matmul(bk[g], mT[:, kc, tok], wout[kc][:, g * 512:(g + 1) * 512],
                                         start=(kc == 0), stop=(kc == 7))
                        post_residual(tt, s, bk, srcname, src_ap, dstname, dst_ap, xsB, junk, ssp, ssum, rs2,
                                      gpost, tmp[s])
                        yield

                def drive(a, b):
                    while a is not None or b is not None:
                        if b is not None:
                            try:
                                next(b)
                            except StopIteration:
                                b = None
                        if a is not None:
                            try:
                                next(a)
                            except StopIteration:
                                a = None

                drive(stage_A(0), None)
                for tt in range(NT):
                    drive(stage_A(tt + 1) if tt + 1 < NT else None, stage_B(tt))
            S.barrier()

        def ffn_phase(l, srcname, src_ap, dstname, dst_ap):
            with ExitStack() as es:
                A_ = lambda n, s, d: alloc(es, n, s, d)
                wup_t = sbt(es, "wup", [128, 8, 2 * DFF], BF16)
                wub_ = [Buf("wup%d" % g) for g in range(4)]
                wup = [V(wup_t[:][:, kc, :], [wub_[kc // 2]]) for kc in range(8)]
                wdn_t = sbt(es, "wdn", [128, NFT, DM], BF16)
                wdb_ = [Buf("wdn%d" % g) for g in range(2)]
                wdn = [V(wdn_t[:][:, kc, :], [wdb_[kc // 11]]) for kc in range(NFT)]
                lp = A_("lp", [128, NLP], F32)
                gpre = A_("gpre", [128, DM], F32)
                gpost = A_("gpost", [128, DM], F32)
                rowv = V(rowp_d[l], [Buf("rowp_d")])
                S.dma("sp", lp, V(lp_d[l], [Buf("lp_d")]))
                S.dma("sp", gpre, rowv[:, 2048:3072].bc([128, 1024]))
                S.dma("sp", gpost, rowv[:, 3072:4096].bc([128, 1024]))
                wv = V(wup_d[l].rearrange("(kc p) n -> p kc n", p=128), [Buf("wup_d")])
                for g in range(4):
                    S.dma("pool", V(wup_t[:][:, 2 * g:2 * g + 2, :], [wub_[g]]), wv[:, 2 * g:2 * g + 2, :])
                wv = V(wdn_d[l].rearrange("(kc p) n -> p kc n", p=128), [Buf("wdn_d")])
                for g in range(2):
                    S.dma("pool", V(wdn_t[:][:, 11 * g:11 * g + 11, :], [wdb_[g]]), wv[:, 11 * g:11 * g + 11, :])
                xs = [A_("xs%d" % s, [128, DM], F32) for s in range(2)]
                hbs = [A_("hbs%d" % s, [128, DM], BF16) for s in range(2)]
                ssv = [A_("ssv%d" % s, [128, 1], F32) for s in range(2)]
                rs = [A_("rs%d" % s, [128, 1], F32) for s in range(2)]
                junk = A_("junk", [128, DM], BF16)
                hT = A_("hT", [128, 8, TT], BF16)
                Bf = [A_("Bf%d" % i, [128, 2, TT + 2], F32) for i in range(2)]
                Hh = A_("Hh", [128, NFT, 2, 2], F32)
                tg = [A_("tg%d" % i, [128, TT], F32) for i in range(2)]
                tu = [A_("tu%d" % i, [128, TT], F32) for i in range(2)]
                sgt = [A_("sgt%d" % i, [128, TT], F32) for i in range(2)]
                actT = A_("actT", [128, NFT, TT], BF16)
                ssp = A_("ssp", [128, 2], F32)
                ssum = A_("ssum", [128, 1], F32)
                rs2 = A_("rs2", [128, 1], F32)
                tmp = [A_("tmp%d" % i, [128, DM], F32) for i in range(2)]
                gen = Rot(banks)
                S.memset("pool", Hh, 0.0)
                fw = lambda i, k: lp[:, LP_FFW + i * 3 + k:LP_FFW + i * 3 + k + 1]
                for tt in range(NT):
                    norm_transpose(tt, srcname, src_ap, xs, ssv, rs, junk, hbs, hT, gpre, gen)
                    for i in range(NFT):
                        bk = gen.get()
                        B = Bf[i % 2]
                        for half, c0 in ((0, i * 128), (1, DFF + i * 128)):
                            for kc in range(8):
                                S.matmul(bk[:, half * TT:(half + 1) * TT], wup[kc][:, c0:c0 + 128], hT[:, kc, :],
                                         start=(kc == 0), stop=(kc == 7))
                        S.copy("pool", B[:, :, 0:2], Hh[:, i, :, :])
                        S.copy("act", B[:, :, 2:2 + TT], bk.re("p (a t) -> p a t", a=2))
                        S.copy("pool", Hh[:, i, :, :], B[:, :, TT:TT + 2])
                        g_, u_ = tg[i % 2], tu[i % 2]
                        S.ts("pool", u_, B[:, 1, 2:2 + TT], fw(NFT + i, 2), ALU.mult)
                        S.ts("dve", g_, B[:, 0, 2:2 + TT], fw(i, 2), ALU.mult)
                        S.stt(u_, B[:, 1, 1:1 + TT], fw(NFT + i, 1), u_, ALU.mult, ALU.add)
                        S.stt(g_, B[:, 0, 1:1 + TT], fw(i, 1), g_, ALU.mult, ALU.add)
                        S.stt(u_, B[:, 1, 0:TT], fw(NFT + i, 0), u_, ALU.mult, ALU.add)
                        S.stt(g_, B[:, 0, 0:TT], fw(i, 0), g_, ALU.mult, ALU.add)
                        S.act(sgt[i % 2], tg[i % 2], AF.Silu)
                        S.tt("pool", actT[:, i, :], sgt[i % 2], tu[i % 2], ALU.mult)
                    for s in range(2):
                        tok = slice(s * 128, (s + 1) * 128)
                        bk = [gen.get(), gen.get()]
                        for g in range(2):
                            for kc in range(NFT):
                                S.matmul(bk[g], actT[:, kc, tok], wdn[kc][:, g * 512:(g + 1) * 512],
                                         start=(kc == 0), stop=(kc == NFT - 1))
                        post_residual(tt, s, bk, srcname, src_ap, dstname, dst_ap, xs, junk, ssp, ssum, rs2,
                                      gpost, tmp[s])
            S.barrier()

        for l in range(NL):
            srcn, srca = ("x", x_d) if l == 0 else ("xb", xb_d)
            mixer_phase(l, srcn, srca, "xa", xa_d)
            if dbg and l == 0 and not int(os.environ.get('KNODX', '0')):
                for tt in range(NT):
                    for s in range(2):
                        r0 = tt * TT + s * 128
                        S.dma("sp", V(dxa_d[r0:r0 + 128, :], [Buf("dxa%d_%d" % (tt, s))]), xview(xa_d, "xa", tt, s))
            dstn, dsta = ("y", y_d) if l == NL - 1 else ("xb", xb_d)
            if not KNOFFN:
                ffn_phase(l, "xa", xa_d, dstn, dsta)
        S.emit()
    return nc, S


def host_layout(inp, NL):
    lp = np.zeros((NL, 128, NLP), np.float32)
    rowp = np.zeros((NL, 1, NROW), np.float32)
    for l in range(NL):
        lp[l, :, LP_DWW:LP_DWW + 62] = inp["conf_dw_w"][l].reshape(31, 2, 128).transpose(2, 1, 0).reshape(128, 62)
        lp[l, :, LP_DWB:LP_DWB + 2] = inp["conf_dw_b"][l].reshape(2, 128).T
        lp[l, :, LP_LNG:LP_LNG + 2] = inp["conf_ln_g"][l].reshape(2, 128).T
        lp[l, :, LP_LNB:LP_LNB + 2] = inp["conf_ln_b"][l].reshape(2, 128).T
        lp[l, :, LP_SCW:LP_SCW + 6] = inp["sc_conv_w"][l].reshape(3, 2, 128).transpose(2, 1, 0).reshape(128, 6)
        lp[l, :, LP_FFW:LP_FFW + 132] = inp["ffn_conv_w"][l].reshape(3, 44, 128).transpose(2, 1, 0).reshape(128, 132)
        rowp[l, 0, 0:1024] = inp["norm_mix_pre"][l]
        rowp[l, 0, 1024:2048] = inp["norm_mix_post"][l]
        rowp[l, 0, 2048:3072] = inp["norm_ffn_pre"][l]
        rowp[l, 0, 3072:4096] = inp["norm_ffn_post"][l]
        rowp[l, 0, 4096:4352] = inp["ret_norm_g"][l]
    return lp, rowp


_CACHE = {}


def run(inp, T, NL, ncores, dbg=False, trace=False, same_engine_sync=True):
    key = (T, NL, dbg, same_engine_sync)
    if key not in _CACHE:
        _CACHE[key] = build(T, NL, dbg, same_engine_sync)
    nc, S = _CACHE[key]
    f = lambda a: np.ascontiguousarray(np.asarray(a, dtype=np.float32))
    lp, rowp = host_layout({k: np.asarray(v) for k, v in inp.items()}, NL)
    con, cs = host_consts(T)
    shared = {
        "w_in": f(inp["w_in"][:NL]), "w_out": f(inp["w_out"][:NL]), "ffn_up": f(inp["ffn_up"][:NL]),
        "ffn_down": f(inp["ffn_down"][:NL]), "lp": lp, "rowp": rowp, "consts": con, "cs": cs,
    }
    x = np.asarray(inp["x"], dtype=np.float32)
    maps = [dict(shared, x=np.ascontiguousarray(x[b, :T])) for b in range(ncores)]
    res = run_bass_kernel_spmd(nc, maps, core_ids=list(range(ncores)), trace=trace)
    return res


def kernel(**inputs):
    res = run(inputs, 4096, 4, 8)
    return np.stack([r["y"] for r in res.results], 0).astype(np.float32)
```

```python
import concourse.bass as bass
import concourse.mybir as mybir

F32 = mybir.dt.float32
BF16 = mybir.dt.bfloat16
AF = mybir.ActivationFunctionType
ALU = mybir.AluOpType
AX = mybir.AxisListType

ENGS = ("pe", "act", "dve", "pool", "sp")


class Buf:
    __slots__ = ("name", "w", "r")

    def __init__(self, name):
        self.name = name
        self.w = []
        self.r = []


class V:
    __slots__ = ("ap", "bufs")

    def __init__(self, ap, bufs):
        self.ap = ap
        self.bufs = tuple(bufs)

    def __getitem__(self, idx):
        return V(self.ap[idx], self.bufs)

    def bc(self, shape):
        return V(self.ap.to_broadcast(shape), self.bufs)

    def re(self, pattern, **kw):
        return V(self.ap.rearrange(pattern, **kw), self.bufs)

    def wb(self, *bufs):
        return V(self.ap, bufs)


class Sched:
    def __init__(self, nc, n_dma_sems=20, same_engine_sync=True):
        self.nc = nc
        self.streams = {e: [] for e in ENGS}
        self.cnt = {e: 0 for e in ENGS}
        self.waited = {e: {} for e in ENGS}
        self.same_engine_sync = same_engine_sync
        self.n_dma_sems = n_dma_sems
        self.dma_val = [0] * n_dma_sems
        self.dma_next = 0
        self.n_ops = 0
        self.n_waits = 0
        self.n_sw = 0

    def _need(self, eng, waits, ev):
        key, val = ev
        if key == eng and (eng == "pe" or not self.same_engine_sync):
            return
        if self.waited[eng].get(key, 0) >= val:
            return
        if waits.get(key, 0) < val:
            waits[key] = val

    def _deps(self, eng, outs, ins, waits):
        for v in ins:
            for b in v.bufs:
                for ev in b.w:
                    self._need(eng, waits, ev)
        for v in outs:
            for b in v.bufs:
                for ev in b.w:
                    self._need(eng, waits, ev)
                for ev in b.r:
                    self._need(eng, waits, ev)

    def _commit(self, eng, waits, outs, ins, ev):
        for k, val in waits.items():
            self.waited[eng][k] = max(self.waited[eng].get(k, 0), val)
        for v in ins:
            for b in v.bufs:
                b.r.append(ev)
        for v in outs:
            for b in v.bufs:
                b.w = [ev]
                b.r = []

    def op(self, eng, fn, outs, ins):
        waits = {}
        self._deps(eng, outs, ins, waits)
        self.cnt[eng] += 1
        ev = (eng, self.cnt[eng])
        self._commit(eng, waits, outs, ins, ev)
        self.streams[eng].append((waits, fn, (eng, 1)))
        self.n_ops += 1
        self.n_waits += len(waits)

    def dma(self, q, out, in_, **kw):
        waits = {}
        self._deps(q, [out], [in_], waits)
        if q == "pool":
            key = ("sw", self.n_sw)
            self.n_sw += 1
            ev = (key, 16)
        else:
            k = self.dma_next
            self.dma_next = (k + 1) % self.n_dma_sems
            key = ("dma", k)
            if self.dma_val[k] > 0:
                self._need(q, waits, (key, self.dma_val[k]))
            self.dma_val[k] += 16
            ev = (key, self.dma_val[k])
        self._commit(q, waits, [out], [in_], ev)
        o_ap, i_ap = out.ap, in_.ap
        self.streams[q].append(
            (waits, lambda e: e.dma_start(out=o_ap, in_=i_ap, **kw), (key, 16)))
        self.n_ops += 1
        self.n_waits += len(waits)
        return ev

    def barrier(self):
        evs = [(e, self.cnt[e]) for e in ENGS if self.cnt[e] > 0]
        evs += [(("dma", k), v) for k, v in enumerate(self.dma_val) if v > 0]
        evs += [(("sw", k), 16) for k in range(self.n_sw)]
        for e in ENGS:
            waits = {}
            for ev in evs:
                if ev[0] == e:
                    continue
                self._need(e, waits, ev)
            if waits:
                for k, val in waits.items():
                    self.waited[e][k] = max(self.waited[e].get(k, 0), val)
                self.streams[e].append((waits, None, None))

    def emit(self, final_events=()):
        nc = self.nc
        from contextlib import ExitStack
        with ExitStack() as es:
            sems = {}
            for e in ENGS:
                sems[e] = es.enter_context(nc.semaphore("s_" + e))
            for k in range(self.n_dma_sems):
                sems[("dma", k)] = es.enter_context(nc.semaphore("s_dma%d" % k))
            for k in range(self.n_sw):
                sems[("sw", k)] = es.enter_context(nc.semaphore("s_sw%d" % k))
            block = es.enter_context(nc.Block())
            streams = self.streams

            def run(engname, eng):
                for waits, fn, inc in streams[engname]:
                    for k, val in waits.items():
                        eng.wait_ge(sems[k], val)
                    if fn is None:
                        continue
                    ins = fn(eng)
                    ins.then_inc(sems[inc[0]], inc[1])

            @block.tensor
            def _(e):
                run("pe", e)

            @block.scalar
            def _(e):
                run("act", e)

            @block.vector
            def _(e):
                run("dve", e)

            @block.gpsimd
            def _(e):
                run("pool", e)

            @block.sync
            def _(e):
                run("sp", e)
                for k in range(self.n_dma_sems):
                    if self.dma_val[k] > 0:
                        e.wait_ge(sems[("dma", k)], self.dma_val[k])
                for k in range(self.n_sw):
                    e.wait_ge(sems[("sw", k)], 16)

    def matmul(self, out, lhsT, rhs, start=True, stop=True, **kw):
        o, l, r = out.ap, lhsT.ap, rhs.ap
        self.op("pe", lambda e: e.matmul(o, l, r, start=start, stop=stop, **kw),
                [out], [lhsT, rhs])

    def transpose(self, out, in_, ident):
        o, i, d = out.ap, in_.ap, ident.ap
        self.op("pe", lambda e: e.transpose(o, i, d), [out], [in_, ident])

    def act(self, out, in_, func, bias=None, scale=None, accum_out=None):
        ins = [in_]
        outs = [out]
        kw = {}
        if bias is not None:
            if isinstance(bias, V):
                ins.append(bias)
                kw["bias"] = bias.ap
            else:
                kw["bias"] = float(bias)
        if scale is not None:
            if isinstance(scale, V):
                ins.append(scale)
                kw["scale"] = scale.ap
            else:
                kw["scale"] = float(scale)
        if accum_out is not None:
            outs.append(accum_out)
            kw["accum_out"] = accum_out.ap
        o, i = out.ap, in_.ap
        self.op("act", lambda e: e.activation(out=o, in_=i, func=func, **kw), outs, ins)

    def tt(self, eng, out, in0, in1, op):
        o, a, b = out.ap, in0.ap, in1.ap
        self.op(eng, lambda e: e.tensor_tensor(out=o, in0=a, in1=b, op=op), [out], [in0, in1])

    def ts(self, eng, out, in0, s1, op0, s2=None, op1=None):
        ins = [in0]
        a1 = s1.ap if isinstance(s1, V) else float(s1)
        if isinstance(s1, V):
            ins.append(s1)
        a2 = None
        if s2 is not None:
            a2 = s2.ap if isinstance(s2, V) else float(s2)
            if isinstance(s2, V):
                ins.append(s2)
        o, a = out.ap, in0.ap
        if op1 is None:
            self.op(eng, lambda e: e.tensor_scalar(out=o, in0=a, scalar1=a1, scalar2=a2, op0=op0),
                    [out], ins)
        else:
            self.op(eng, lambda e: e.tensor_scalar(out=o, in0=a, scalar1=a1, scalar2=a2, op0=op0, op1=op1),
                    [out], ins)

    def stt(self, out, in0, scalar, in1, op0, op1):
        ins = [in0, in1]
        sc = scalar.ap if isinstance(scalar, V) else float(scalar)
        if isinstance(scalar, V):
            ins.append(scalar)
        o, a, b = out.ap, in0.ap, in1.ap
        self.op("dve", lambda e: e.scalar_tensor_tensor(out=o, in0=a, scalar=sc, in1=b, op0=op0, op1=op1),
                [out], ins)

    def copy(self, eng, out, in_):
        o, i = out.ap, in_.ap
        if eng == "act":
            self.op("act", lambda e: e.copy(out=o, in_=i), [out], [in_])
        else:
            self.op(eng, lambda e: e.tensor_copy(out=o, in_=i), [out], [in_])

    def reduce(self, eng, out, in_, op, axis=AX.X):
        o, i = out.ap, in_.ap
        self.op(eng, lambda e: e.tensor_reduce(out=o, in_=i, axis=axis, op=op), [out], [in_])

    def memset(self, eng, out, val):
        o = out.ap
        self.op(eng, lambda e: e.memset(o, val), [out], [])

import numpy as np
from contextlib import ExitStack
from concourse.bass_utils import run_bass_kernel_spmd

DM = 1024
DIN = 3072
DFF = 2816
NFT = 22
TT = 256
EPS = 1e-6
NLP = 62 + 2 + 2 + 2 + 6 + 132
LP_DWW, LP_DWB, LP_LNG, LP_LNB, LP_SCW, LP_FFW = 0, 62, 64, 66, 68, 74
NROW = 4 * 1024 + 256
C_ID, C_M1, C_CAUS, C_MASK, C_QKD, C_CD = 0, 128, 256, 384, 384 + 512, 384 + 512 + 8
NCON = C_CD + 2


def host_consts(T):
    c = np.zeros((128, NCON), np.float32)
    j = np.arange(128)
    c[:, C_ID:C_ID + 128] = np.eye(128)
    c[:, C_M1:C_M1 + 128] = -(j[:, None] >= j[None, :]).astype(np.float32)
    c[:, C_CAUS:C_CAUS + 128] = (j[None, :] >= j[:, None]).astype(np.float32)
    t = np.arange(256)
    for r in range(2):
        c[:, C_MASK + r * 256:C_MASK + (r + 1) * 256] = ((128 * r + j)[:, None] < t[None, :]).astype(np.float32)
    lg = np.log1p(-np.exp2(-5.0 - np.arange(4, dtype=np.float64)))
    for h in range(4):
        c[:, C_QKD + h] = np.exp((j + 1.0) * lg[h])
        c[:, C_QKD + 4 + h] = np.exp(-(j + 1.0) * lg[h]) * 0.125
    for idx in range(2):
        for half in range(2):
            c[half * 64:(half + 1) * 64, C_CD + idx] = np.exp(128.0 * lg[2 * idx + half])
    pos = np.arange(T, dtype=np.float32)
    inv = (10000.0 ** (-np.arange(0, 64, 2, dtype=np.float32) / 64)).astype(np.float32)
    ang = (pos[:, None] * inv[None, :]).astype(np.float32)
    cs = np.concatenate([np.cos(ang), np.sin(ang)], -1).astype(np.float32)
    cs = np.ascontiguousarray(cs.reshape(T // 128, 128, 64).transpose(1, 0, 2))
    return c, cs


def build(T, NL, dbg=False, same_engine_sync=True):
    import os
    KSTOP = int(os.environ.get("KSTOP", "99"))
    KNOFFN = int(os.environ.get("KNOFFN", "0"))
    NT = T // TT
    NB = T // 128
    nc = bass.Bass("TRN2", target_bir_lowering=False)
    dram = lambda n, s, k="ExternalInput", dt=F32: nc.dram_tensor(n, list(s), dt, kind=k).ap()
    x_d = dram("x", [T, DM])
    win_d = dram("w_in", [NL, DM, DIN])
    wout_d = dram("w_out", [NL, DM, DM])
    wup_d = dram("ffn_up", [NL, DM, 2 * DFF])
    wdn_d = dram("ffn_down", [NL, DFF, DM])
    lp_d = dram("lp", [NL, 128, NLP])
    rowp_d = dram("rowp", [NL, 1, NROW])
    con_d = dram("consts", [128, NCON])
    cs_d = dram("cs", [128, NB, 64])
    y_d = dram("y", [T, DM], "ExternalOutput")
    xa_d = dram("xa", [T, DM], "Internal")
    xb_d = dram("xb", [T, DM], "Internal")
    if dbg:
        dmix_d = dram("dbg_mix", [NT, 128, 8, TT], "ExternalOutput", BF16)
        dxa_d = dram("dbg_xa", [T, DM], "ExternalOutput")

    S = Sched(nc, same_engine_sync=same_engine_sync)
    xbufs = {}

    def xview(ap, name, tt, s):
        bl = xbufs.setdefault(name, [Buf("%s%d" % (name, i)) for i in range(NT)])
        r0 = tt * TT + s * 128
        return V(ap[r0:r0 + 128, :], [bl[tt]])

    with ExitStack() as top:
        uid = [0]

        def sbt(es, name, shape, dt):
            uid[0] += 1
            return es.enter_context(nc.sbuf_tensor("%s_u%d" % (name, uid[0]), list(shape), dt))

        def alloc(es, name, shape, dt):
            t = sbt(es, name, shape, dt)
            return V(t[:], [Buf(name)])

        banks = []
        for i in range(8):
            t = top.enter_context(nc.psum_tensor("bank%d" % i, [128, 512], F32))
            banks.append(V(t[:], [Buf("bank%d" % i)]))

        class Rot:
            def __init__(self, items):
                self.items = items
                self.i = 0

            def get(self):
                v = self.items[self.i % len(self.items)]
                self.i += 1
                return v

        def bfview(bank):
            return V(bank.ap.bitcast(BF16), bank.bufs)

        con = alloc(top, "con", [128, NCON], F32)
        S.dma("sp", con, V(con_d, [Buf("con_d")]))
        identb = alloc(top, "identb", [128, 128], BF16)
        negm1 = alloc(top, "negm1", [128, 128], BF16)
        negones = alloc(top, "negones", [128, 128], BF16)
        onesm = alloc(top, "onesm", [128, 128], BF16)
        S.copy("dve", identb, con[:, C_ID:C_ID + 128])
        S.copy("dve", negm1, con[:, C_M1:C_M1 + 128])
        S.memset("dve", negones, -1.0)
        S.memset("dve", onesm, 1.0 / 256)
        identf = con[:, C_ID:C_ID + 128]
        caus = con[:, C_CAUS:C_CAUS + 128]
        maskd = [con[:, C_MASK + r * 256:C_MASK + (r + 1) * 256] for r in range(2)]
        qkd = con[:, C_QKD:C_QKD + 8]
        cdt = con[:, C_CD:C_CD + 2]
        cs_dv = V(cs_d, [Buf("cs_d")])

        def norm_transpose(tt, srcname, src_ap, xs, ssv, rs, junk, hbs, hT, gpre, gen):
            for s in range(2):
                S.dma("sp", xs[s], xview(src_ap, srcname, tt, s))
                S.act(hbs[s], xs[s], AF.Square, accum_out=ssv[s])
                S.act(rs[s], ssv[s], AF.Ln, scale=1.0 / DM, bias=EPS)
                S.act(rs[s], rs[s], AF.Exp, scale=-0.5)
                S.stt(hbs[s], xs[s], rs[s], gpre, ALU.mult, ALU.mult)
                pb = bfview(gen.get())
                for kc in range(8):
                    S.transpose(pb[:, kc * 128:(kc + 1) * 128], hbs[s][:, kc * 128:(kc + 1) * 128], identb)
                S.copy("act" if s == 0 else "dve", hT[:, :, s * 128:(s + 1) * 128],
                       pb.re("p (a b) -> p a b", b=128))

        def post_residual(tt, s, bk, srcname, src_ap, dstname, dst_ap, xs, junk, ssp, ssum, rs2, gpost, tmp):
            for g in range(2):
                S.act(junk[:, 0:512], bk[g], AF.Square, accum_out=ssp[:, g:g + 1])
            S.tt("dve", ssum, ssp[:, 0:1], ssp[:, 1:2], ALU.add)
            S.act(rs2, ssum, AF.Ln, scale=1.0 / DM, bias=EPS)
            S.act(rs2, rs2, AF.Exp, scale=-0.5)
            S.dma("sp", xs[s], xview(src_ap, srcname, tt, s))
            for g in range(2):
                S.stt(tmp[:, g * 512:(g + 1) * 512], bk[g], rs2, gpost[:, g * 512:(g + 1) * 512],
                      ALU.mult, ALU.mult)
            S.tt("pool", tmp, tmp, xs[s], ALU.add)
            S.dma("sp", xview(dst_ap, dstname, tt, s), tmp)

        def mixer_phase(l, srcname, src_ap, dstname, dst_ap):
            with ExitStack() as es:
                A_ = lambda n, s, d: alloc(es, n, s, d)
                win_t = sbt(es, "win", [128, 8, DIN], BF16)
                wb_ = [Buf("winA"), Buf("winB")]
                win = [V(win_t[:][:, kc, :], [wb_[kc // 4]]) for kc in range(8)]
                wout_t = sbt(es, "wout", [128, 8, DM], BF16)
                wob_ = Buf("woutA")
                wout = [V(wout_t[:][:, kc, :], [wob_]) for kc in range(8)]
                lp = A_("lp", [128, NLP], F32)
                gpre = A_("gpre", [128, DM], F32)
                gpost = A_("gpost", [128, DM], F32)
                retg = A_("retg", [128, 256], F32)
                diag = A_("diag", [128, 62, 128], BF16)
                rowv = V(rowp_d[l], [Buf("rowp_d")])
                S.dma("sp", lp, V(lp_d[l], [Buf("lp_d")]))
                S.dma("sp", gpre, rowv[:, 0:1024].bc([128, 1024]))
                S.dma("sp", gpost, rowv[:, 1024:2048].bc([128, 1024]))
                S.dma("sp", retg, rowv[:, 4096:4352].bc([128, 256]))
                wv = V(win_d[l].rearrange("(kc p) n -> p kc n", p=128), [Buf("win_d")])
                for g in range(2):
                    S.dma("pool", V(win_t[:][:, 4 * g:4 * g + 4, :], [wb_[g]]), wv[:, 4 * g:4 * g + 4, :])
                wv = V(wout_d[l].rearrange("(kc p) n -> p kc n", p=128), [Buf("wout_d")])
                S.dma("pool", V(wout_t[:], [wob_]), wv)
                for i in range(62):
                    S.ts("dve" if i % 2 else "pool", diag[:, i, :], identf, lp[:, LP_DWW + i:LP_DWW + i + 1], ALU.mult)

                xs = [A_("xs%d" % s, [128, DM], F32) for s in range(2)]
                hbs = [A_("hbs%d" % s, [128, DM], BF16) for s in range(2)]
                ssv = [A_("ssv%d" % s, [128, 1], F32) for s in range(2)]
                rs = [A_("rs%d" % s, [128, 1], F32) for s in range(2)]
                junk = A_("junk", [128, 512], BF16)
                hT = A_("hT", [128, 8, TT], BF16)
                cst = A_("cst", [128, 2, 64], F32)
                kT_t = sbt(es, "kT", [128, 2, T], BF16)
                kTb = [Buf("kT%d" % i) for i in range(NT)]
                vc_t = sbt(es, "vc", [128, NB, 256], BF16)
                vcb = [Buf("vc%d" % i) for i in range(NT)]
                qTs = [A_("qT%d" % i, [128, 2, TT], BF16) for i in range(2)]
                xsB1 = A_("xsB", [128, DM], F32)
                xsB = [xsB1, xsB1]
                Ub = [[A_("Ub%d%d" % (ct, p), [128, 30 + TT], BF16) for p in range(2)] for ct in range(2)]
                sgm = A_("sgm", [128, TT], F32)
                cvb = A_("cvb", [128, 2, TT], F32)
                cvh = A_("cvh", [128, 2, TT], BF16)
                sqb = A_("sqb", [128, 2, TT], BF16)
                mean_sb = A_("mean_sb", [128, TT], F32)
                var_sb = A_("var_sb", [128, TT], F32)
                ytmp = A_("ytmp", [128, TT], F32)
                Csb = A_("Csb", [128, TT], F32)
                Bsb = A_("Bsb", [128, 2, TT], BF16)
                Pb = [A_("Pb%d" % ct, [128, 2 + TT], F32) for ct in range(2)]
                tcv = A_("tcv", [128, TT], F32)
                r32 = A_("r32", [128, 512], F32)
                rot = A_("rot", [128, 512], F32)
                rta = A_("rta", [128, 256], F32)
                rtb = A_("rtb", [128, 256], F32)
                rtc = A_("rtc", [128, 256], F32)
                rtd = A_("rtd", [128, 256], F32)
                rqkb = A_("rqkb", [128, 512], BF16)
                rqkT = A_("rqkT", [128, 4, 128], BF16)
                rvb = A_("rvb", [128, 256], BF16)
                sgate = A_("sgate", [128, 256], F32)
                innD = A_("innD", [128, 512], BF16)
                osb = A_("osb", [128, 256], F32)
                osq = A_("osq", [128, 256], F32)
                st4 = [A_("st4_%d" % i, [128, 4], F32) for i in range(4)]
                yn = A_("yn", [128, 256], F32)
                ybf = A_("ybf", [128, 256], BF16)
                S32 = A_("S32", [128, 2, 64], F32)
                Sbf = A_("Sbf", [128, 2, 64], BF16)
                e_ = [[A_("e%d_%d" % (i, h), [128, TT], F32) for h in range(2)] for i in range(3)]
                sp_ = [[A_("sp%d_%d" % (i, h), [128, TT], BF16) for h in range(2)] for i in range(2)]
                Aacc = [A_("Aacc%d" % h, [128, TT], BF16) for h in range(2)]
                E2 = [A_("E2_%d" % h, [128, TT], F32) for h in range(2)]
                attn = [[A_("attn%d_%d" % (i, h), [128, TT], BF16) for h in range(2)] for i in range(2)]
                mixTs = [A_("mixT%d" % i, [128, 8, TT], BF16) for i in range(2)]
                ssp = A_("ssp", [128, 2], F32)
                ssum = A_("ssum", [128, 1], F32)
                rs2 = A_("rs2", [128, 1], F32)
                tmp1 = A_("tmp", [128, DM], F32)
                tmp = [tmp1, tmp1]

                gen = Rot(banks[0:2])
                zsl = [banks[2 + hh][:, 0:TT] for hh in range(2)]
                tsl = [banks[4 + hh][:, 0:TT] for hh in range(2)]
                posl = [banks[6 + hh][hh * 64:hh * 64 + 64, :] for hh in range(2)]
                for ct in range(2):
                    S.memset("pool", Ub[ct][1][:, TT:TT + 30], 0.0)
                    S.memset("pool", Pb[ct][:, TT:TT + 2], 0.0)
                S.memset("pool", S32, 0.0)
                S.memset("pool", Sbf, 0.0)

                def proj_feat(dst, col0):
                    for kc in range(8):
                        S.matmul(dst, win[kc][:, col0:col0 + 128], hT[:, kc, :], start=(kc == 0), stop=(kc == 7))

                def stage_A(tt):
                    mT = mixTs[tt % 2]
                    qTt = qTs[tt % 2]
                    t0 = tt * TT
                    par = tt % 2
                    kTv = V(kT_t[:][:, :, t0:t0 + TT], [kTb[tt]])
                    norm_transpose(tt, srcname, src_ap, xs, ssv, rs, junk, hbs, hT, gpre, gen)
                    S.dma("sp", cst, cs_dv[:, 2 * tt:2 * tt + 2, :])
                    yield
                    for ct in range(2):
                        bk = gen.get()
                        proj_feat(bk[:, 0:TT], ct * 128)
                        proj_feat(bk[:, TT:2 * TT], 256 + ct * 128)
                        S.act(sgm, bk[:, TT:2 * TT], AF.Sigmoid)
                        S.copy("pool", Ub[ct][par][:, 0:30], Ub[ct][1 - par][:, TT:TT + 30])
                        S.tt("dve", Ub[ct][par][:, 30:30 + TT], bk[:, 0:TT], sgm, ALU.mult)
                        yield
                    yield
                    for ct in range(2):
                        bk = gen.get()
                        proj_feat(bk[:, 0:TT], 512 + ct * 128)
                        proj_feat(bk[:, TT:2 * TT], 768 + ct * 128)
                        S.ts("dve", qTt[:, ct, :], bk[:, 0:TT], 0.125, ALU.mult)
                        S.copy("dve", kTv[:, ct, :], bk[:, TT:2 * TT])
                        yield
                    yield
                    for ct in range(2):
                        bk = gen.get()
                        bk2 = gen.get()
                        proj_feat(bk[:, 0:TT], 2304 + ct * 128)
                        proj_feat(bk[:, TT:2 * TT], 2560 + ct * 128)
                        proj_feat(bk2[:, 0:TT], 2816 + ct * 128)
                        S.copy("act", Bsb[:, ct, :], bk[:, 0:TT])
                        S.copy("act", Csb, bk[:, TT:2 * TT])
                        S.copy("pool", Pb[ct][:, 0:2], Pb[ct][:, TT:TT + 2])
                        S.tt("dve", Pb[ct][:, 2:2 + TT], bk2[:, 0:TT], Csb, ALU.mult)
                        yield
                    yield
                    for ct in range(2):
                        bk = gen.get()
                        for k in range(31):
                            S.matmul(bk[:, 0:TT], diag[:, ct * 31 + k, :], Ub[ct][par][:, k:k + TT],
                                     start=(k == 0), stop=(k == 30))
                        S.ts("dve", cvb[:, ct, :], bk[:, 0:TT], lp[:, LP_DWB + ct:LP_DWB + ct + 1], ALU.add)
                        S.act(sqb[:, ct, :], cvb[:, ct, :], AF.Square)
                        S.copy("dve", cvh[:, ct, :], cvb[:, ct, :])
                        yield
                    bk = gen.get()
                    for ct in range(2):
                        S.matmul(bk[:, 0:TT], onesm, cvh[:, ct, :], start=(ct == 0), stop=(ct == 1))
                    for ct in range(2):
                        S.matmul(bk[:, TT:2 * TT], onesm, sqb[:, ct, :], start=(ct == 0), stop=(ct == 1))
                    S.copy("act", mean_sb, bk[:, 0:TT])
                    S.tt("dve", var_sb, mean_sb, mean_sb, ALU.mult)
                    S.tt("dve", var_sb, bk[:, TT:2 * TT], var_sb, ALU.subtract)
                    S.act(var_sb, var_sb, AF.Ln, bias=EPS)
                    S.act(var_sb, var_sb, AF.Exp, scale=-0.5)
                    for ct in range(2):
                        S.tt("dve", ytmp, cvb[:, ct, :], mean_sb, ALU.subtract)
                        S.tt("dve", ytmp, ytmp, var_sb, ALU.mult)
                        S.act(mT[:, ct, :], ytmp, AF.Silu, scale=lp[:, LP_LNG + ct:LP_LNG + ct + 1],
                              bias=lp[:, LP_LNB + ct:LP_LNB + ct + 1])
                    yield
                    for ct in range(2):
                        w = lambda k: lp[:, LP_SCW + ct * 3 + k:LP_SCW + ct * 3 + k + 1]
                        S.ts("dve", tcv, Pb[ct][:, 2:2 + TT], w(2), ALU.mult)
                        S.stt(tcv, Pb[ct][:, 1:1 + TT], w(1), tcv, ALU.mult, ALU.add)
                        S.stt(tcv, Pb[ct][:, 0:TT], w(0), tcv, ALU.mult, ALU.add)
                        S.tt("dve", mT[:, 6 + ct, :], tcv, Bsb[:, ct, :], ALU.mult)
                    yield
                    for s in range(2):
                        cb = tt * 2 + s
                        tok = slice(s * 128, (s + 1) * 128)
                        bA = gen.get()
                        for kc in range(8):
                            S.matmul(bA, hT[:, kc, tok], win[kc][:, 1280:1792], start=(kc == 0), stop=(kc == 7))
                        S.copy("act", r32, bA)
                        bB = gen.get()
                        for kc in range(8):
                            S.matmul(bB, hT[:, kc, tok], win[kc][:, 1792:2304], start=(kc == 0), stop=(kc == 7))
                        S.copy("act", rvb, bB[:, 0:256])
                        S.act(sgate, bB[:, 256:512], AF.Silu)
                        bC = gen.get()
                        for kc in range(8):
                            S.matmul(bC[:, 0:256], hT[:, kc, tok], win[kc][:, 1024:1280], start=(kc == 0), stop=(kc == 7))
                        S.copy("act", V(vc_t[:][:, cb, :], [vcb[tt]]), bC[:, 0:256])
                        yield
                        xv = r32.re("p (g two f) -> p g two f", two=2, f=32)
                        ov = rot.re("p (g two f) -> p g two f", two=2, f=32)
                        x1, x2 = xv[:, :, 0, :], xv[:, :, 1, :]
                        cosb = cst[:, s, 0:32].re("p (o f) -> p o f", o=1).bc([128, 8, 32])
                        sinb = cst[:, s, 32:64].re("p (o f) -> p o f", o=1).bc([128, 8, 32])
                        v3 = lambda t: t.re("p (g f) -> p g f", f=32)
                        S.tt("dve", v3(rta), x1, cosb, ALU.mult)
                        S.tt("dve", v3(rtb), x2, sinb, ALU.mult)
                        S.tt("dve", ov[:, :, 0, :], v3(rta), v3(rtb), ALU.subtract)
                        S.tt("pool", v3(rtc), x2, cosb, ALU.mult)
                        S.tt("pool", v3(rtd), x1, sinb, ALU.mult)
                        S.tt("pool", ov[:, :, 1, :], v3(rtc), v3(rtd), ALU.add)
                        S.tt("dve", rqkb.re("p (g f) -> p g f", f=64), rot.re("p (g f) -> p g f", f=64),
                             qkd.re("p (g o) -> p g o", o=1).bc([128, 8, 64]), ALU.mult)
                        pb = bfview(gen.get())
                        for j in range(4):
                            S.transpose(pb[:, j * 128:(j + 1) * 128], rqkb[:, j * 128:(j + 1) * 128], identb)
                        S.copy("act", rqkT, pb[:, 0:512].re("p (a b) -> p a b", b=128))
                        yield
                        bI = [gen.get(), gen.get()]
                        for h in range(4):
                            pr = slice((h % 2) * 64, (h % 2) * 64 + 64)
                            S.matmul(bI[h % 2][:, (h // 2) * 128:(h // 2) * 128 + 128], rqkT[pr, 2 + h // 2, :],
                                     rqkT[pr, h // 2, :])
                        for par2 in range(2):
                            S.tt("dve", innD[:, par2 * 256:(par2 + 1) * 256].re("p (h q) -> p h q", q=128),
                                 bI[par2][:, 0:256].re("p (h q) -> p h q", q=128),
                                 caus.re("p (o q) -> p o q", o=1).bc([128, 2, 128]), ALU.mult)
                        bO = [gen.get(), gen.get()]
                        for h in range(4):
                            pr = slice((h % 2) * 64, (h % 2) * 64 + 64)
                            j = (h % 2) * 2 + h // 2
                            dst = bO[h % 2][:, (h // 2) * 64:(h // 2) * 64 + 64]
                            S.matmul(dst, innD[:, j * 128:(j + 1) * 128],
                                     rvb[:, h * 64:(h + 1) * 64], start=True, stop=False)
                            S.matmul(dst, rqkT[pr, h // 2, :], Sbf[pr, h // 2, :],
                                     start=False, stop=True)
                        for par2 in range(2):
                            S.copy("act", osb.re("p (a b e) -> p a b e", b=2, e=64)[:, :, par2, :],
                                   bO[par2][:, 0:128].re("p (a e) -> p a e", e=64))
                        bS = gen.get()
                        for h in range(4):
                            pr = slice((h % 2) * 64, (h % 2) * 64 + 64)
                            S.matmul(bS[pr, (h // 2) * 64:(h // 2) * 64 + 64], rqkb[:, 256 + h * 64:256 + (h + 1) * 64],
                                     rvb[:, h * 64:(h + 1) * 64])
                        S.tt("dve", S32, S32, bS[:, 0:128].re("p (a e) -> p a e", e=64), ALU.add)
                        S.tt("dve", S32, S32, cdt.re("p (a o) -> p a o", o=1).bc([128, 2, 64]), ALU.mult)
                        S.copy("dve", Sbf, S32)
                        yield
                        S.act(osq, osb, AF.Square)
                        o3 = lambda t: t.re("p (h e) -> p h e", e=64)
                        S.reduce("dve", st4[0], o3(osb), ALU.add)
                        S.reduce("dve", st4[1], o3(osq), ALU.add)
                        S.ts("dve", st4[0], st4[0], 1.0 / 64, ALU.mult)
                        S.tt("dve", st4[2], st4[0], st4[0], ALU.mult)
                        S.stt(st4[3], st4[1], 1.0 / 64, st4[2], ALU.mult, ALU.subtract)
                        S.act(st4[3], st4[3], AF.Ln, bias=EPS)
                        S.act(st4[3], st4[3], AF.Exp, scale=-0.5)
                        b4 = lambda t: t.re("p (h o) -> p h o", o=1).bc([128, 4, 64])
                        S.tt("dve", o3(yn), o3(osb), b4(st4[0]), ALU.subtract)
                        S.tt("dve", o3(yn), o3(yn), b4(st4[3]), ALU.mult)
                        S.tt("pool", yn, yn, retg, ALU.mult)
                        S.tt("pool", ybf, yn, sgate, ALU.mult)
                        pb = bfview(gen.get())
                        for j in range(2):
                            S.transpose(pb[:, j * 128:(j + 1) * 128], ybf[:, j * 128:(j + 1) * 128], identb)
                        S.copy("act", mT[:, 4:6, tok], pb[:, 0:256].re("p (a b) -> p a b", b=128))
                        yield

                def stage_B(tt):
                    mT = mixTs[tt % 2]
                    qTt = qTs[tt % 2]
                    nkb = 2 * tt + 2
                    kbs = list(range(nkb - 1, -1, -1))
                    for hp in range(2):
                        def st1a(n):
                            kb = kbs[n]
                            for hh in range(2):
                                pr = slice(hh * 64, hh * 64 + 64)
                                kblk = V(kT_t[:][pr, hp, kb * 128:(kb + 1) * 128], [kTb[kb // 2]])
                                S.matmul(zsl[hh], kblk, qTt[pr, hp, :])

                        def st1b(n):
                            for hh in range(2):
                                S.act(e_[n % 3][hh], zsl[hh], AF.Exp)

                        def st2(n):
                            kb = kbs[n]
                            r = kb - 2 * tt
                            first, last = (n == 0), (kb == 0)
                            for hh in range(2):
                                e, sp = e_[n % 3][hh], sp_[n % 2][hh]
                                S.act(sp, e, AF.Ln, bias=1.0)
                                if r >= 0:
                                    S.tt("pool", sp, sp, maskd[r], ALU.mult)
                                    S.tt("pool", e, e, maskd[r], ALU.mult)
                            for hh in range(2):
                                S.matmul(tsl[hh], negm1, sp_[n % 2][hh], start=True, stop=first)
                                if not first:
                                    S.matmul(tsl[hh], negones, Aacc[hh], start=False, stop=True)
                            if not last:
                                for hh in range(2):
                                    if first:
                                        S.copy("pool", Aacc[hh], sp_[n % 2][hh])
                                    else:
                                        S.tt("pool", Aacc[hh], Aacc[hh], sp_[n % 2][hh], ALU.add)

                        def st3(n):
                            kb = kbs[n]
                            first, last = (n == 0), (kb == 0)
                            for hh in range(2):
                                S.act(E2[hh], tsl[hh], AF.Exp)
                                S.tt("dve", attn[n % 2][hh], e_[n % 3][hh], E2[hh], ALU.mult)
                            for hh in range(2):
                                h = 2 * hp + hh
                                vblk = V(vc_t[:][:, kb, h * 64:(h + 1) * 64], [vcb[kb // 2]])
                                S.matmul(posl[hh][:, hp * TT:(hp + 1) * TT], vblk, attn[n % 2][hh],
                                         start=first, stop=last)

                        for itn in range(nkb + 2):
                            if itn < nkb:
                                st1a(itn)
                            if itn - 2 >= 0:
                                st3(itn - 2)
                            if 0 <= itn - 1 < nkb:
                                st2(itn - 1)
                            if itn < nkb:
                                st1b(itn)
                            yield
                        for hh in range(2):
                            pr = slice(hh * 64, hh * 64 + 64)
                            S.copy("act", mT[pr, 2 + hp, :], posl[hh][:, hp * TT:(hp + 1) * TT])
                    yield
                    if dbg:
                        S.dma("sp", V(dmix_d[tt], [Buf("dmix%d" % tt)]), mT)
                    for s in range(2):
                        tok = slice(s * 128, (s + 1) * 128)
                        bk = [gen.get(), gen.get()]
                        for g in range(2):
                            for kc in range(8):
                                S.matmul(bk[g], mT[:, kc, tok], wout[kc][:, g * 512:(g + 1) * 512],
                                         start=(kc == 0), stop=(kc == 7))
                        post_residual(tt, s, bk, srcname, src_ap, dstname, dst_ap, xsB, junk, ssp, ssum, rs2,
                                      gpost, tmp[s])
                        yield

                def drive(a, b):
                    while a is not None or b is not None:
                        if b is not None:
                            try:
                                next(b)
                            except StopIteration:
                                b = None
                        if a is not None:
                            try:
                                next(a)
                            except StopIteration:
                                a = None

                drive(stage_A(0), None)
                for tt in range(NT):
                    drive(stage_A(tt + 1) if tt + 1 < NT else None, stage_B(tt))
            S.barrier()

        def ffn_phase(l, srcname, src_ap, dstname, dst_ap):
            with ExitStack() as es:
                A_ = lambda n, s, d: alloc(es, n, s, d)
                wup_t = sbt(es, "wup", [128, 8, 2 * DFF], BF16)
                wub_ = [Buf("wup%d" % g) for g in range(4)]
                wup = [V(wup_t[:][:, kc, :], [wub_[kc // 2]]) for kc in range(8)]
                wdn_t = sbt(es, "wdn", [128, NFT, DM], BF16)
                wdb_ = [Buf("wdn%d" % g) for g in range(2)]
                wdn = [V(wdn_t[:][:, kc, :], [wdb_[kc // 11]]) for kc in range(NFT)]
                lp = A_("lp", [128, NLP], F32)
                gpre = A_("gpre", [128, DM], F32)
                gpost = A_("gpost", [128, DM], F32)
                rowv = V(rowp_d[l], [Buf("rowp_d")])
                S.dma("sp", lp, V(lp_d[l], [Buf("lp_d")]))
                S.dma("sp", gpre, rowv[:, 2048:3072].bc([128, 1024]))
                S.dma("sp", gpost, rowv[:, 3072:4096].bc([128, 1024]))
                wv = V(wup_d[l].rearrange("(kc p) n -> p kc n", p=128), [Buf("wup_d")])
                for g in range(4):
                    S.dma("pool", V(wup_t[:][:, 2 * g:2 * g + 2, :], [wub_[g]]), wv[:, 2 * g:2 * g + 2, :])
                wv = V(wdn_d[l].rearrange("(kc p) n -> p kc n", p=128), [Buf("wdn_d")])
                for g in range(2):
                    S.dma("pool", V(wdn_t[:][:, 11 * g:11 * g + 11, :], [wdb_[g]]), wv[:, 11 * g:11 * g + 11, :])
                xs = [A_("xs%d" % s, [128, DM], F32) for s in range(2)]
                hbs = [A_("hbs%d" % s, [128, DM], BF16) for s in range(2)]
                ssv = [A_("ssv%d" % s, [128, 1], F32) for s in range(2)]
                rs = [A_("rs%d" % s, [128, 1], F32) for s in range(2)]
                junk = A_("junk", [128, DM], BF16)
                hT = A_("hT", [128, 8, TT], BF16)
                Bf = [A_("Bf%d" % i, [128, 2, TT + 2], F32) for i in range(2)]
                Hh = A_("Hh", [128, NFT, 2, 2], F32)
                tg = [A_("tg%d" % i, [128, TT], F32) for i in range(2)]
                tu = [A_("tu%d" % i, [128, TT], F32) for i in range(2)]
                sgt = [A_("sgt%d" % i, [128, TT], F32) for i in range(2)]
                actT = A_("actT", [128, NFT, TT], BF16)
                ssp = A_("ssp", [128, 2], F32)
                ssum = A_("ssum", [128, 1], F32)
                rs2 = A_("rs2", [128, 1], F32)
                tmp = [A_("tmp%d" % i, [128, DM], F32) for i in range(2)]
                gen = Rot(banks)
                S.memset("pool", Hh, 0.0)
                fw = lambda i, k: lp[:, LP_FFW + i * 3 + k:LP_FFW + i * 3 + k + 1]
                for tt in range(NT):
                    norm_transpose(tt, srcname, src_ap, xs, ssv, rs, junk, hbs, hT, gpre, gen)
                    for i in range(NFT):
                        bk = gen.get()
                        B = Bf[i % 2]
                        for half, c0 in ((0, i * 128), (1, DFF + i * 128)):
                            for kc in range(8):
                                S.matmul(bk[:, half * TT:(half + 1) * TT], wup[kc][:, c0:c0 + 128], hT[:, kc, :],
                                         start=(kc == 0), stop=(kc == 7))
                        S.copy("pool", B[:, :, 0:2], Hh[:, i, :, :])
                        S.copy("act", B[:, :, 2:2 + TT], bk.re("p (a t) -> p a t", a=2))
                        S.copy("pool", Hh[:, i, :, :], B[:, :, TT:TT + 2])
                        g_, u_ = tg[i % 2], tu[i % 2]
                        S.ts("dve", g_, B[:, 0, 2:2 + TT], fw(i, 2), ALU.mult)
                        S.ts("dve", u_, B[:, 1, 2:2 + TT], fw(NFT + i, 2), ALU.mult)
                        S.stt(g_, B[:, 0, 1:1 + TT], fw(i, 1), g_, ALU.mult, ALU.add)
                        S.stt(u_, B[:, 1, 1:1 + TT], fw(NFT + i, 1), u_, ALU.mult, ALU.add)
                        S.stt(g_, B[:, 0, 0:TT], fw(i, 0), g_, ALU.mult, ALU.add)
                        S.stt(u_, B[:, 1, 0:TT], fw(NFT + i, 0), u_, ALU.mult, ALU.add)
                        S.act(sgt[i % 2], tg[i % 2], AF.Silu)
                        S.tt("pool", actT[:, i, :], sgt[i % 2], tu[i % 2], ALU.mult)
                    for s in range(2):
                        tok = slice(s * 128, (s + 1) * 128)
                        bk = [gen.get(), gen.get()]
                        for g in range(2):
                            for kc in range(NFT):
                                S.matmul(bk[g], actT[:, kc, tok], wdn[kc][:, g * 512:(g + 1) * 512],
                                         start=(kc == 0), stop=(kc == NFT - 1))
                        post_residual(tt, s, bk, srcname, src_ap, dstname, dst_ap, xs, junk, ssp, ssum, rs2,
                                      gpost, tmp[s])
            S.barrier()

        for l in range(NL):
            srcn, srca = ("x", x_d) if l == 0 else ("xb", xb_d)
            mixer_phase(l, srcn, srca, "xa", xa_d)
            if dbg and l == 0 and not int(os.environ.get('KNODX', '0')):
                for tt in range(NT):
                    for s in range(2):
                        r0 = tt * TT + s * 128
                        S.dma("sp", V(dxa_d[r0:r0 + 128, :], [Buf("dxa%d_%d" % (tt, s))]), xview(xa_d, "xa", tt, s))
            dstn, dsta = ("y", y_d) if l == NL - 1 else ("xb", xb_d)
            if not KNOFFN:
                ffn_phase(l, "xa", xa_d, dstn, dsta)
        S.emit()
    return nc, S


def host_layout(inp, NL):
    lp = np.zeros((NL, 128, NLP), np.float32)
    rowp = np.zeros((NL, 1, NROW), np.float32)
    for l in range(NL):
        lp[l, :, LP_DWW:LP_DWW + 62] = inp["conf_dw_w"][l].reshape(31, 2, 128).transpose(2, 1, 0).reshape(128, 62)
        lp[l, :, LP_DWB:LP_DWB + 2] = inp["conf_dw_b"][l].reshape(2, 128).T
        lp[l, :, LP_LNG:LP_LNG + 2] = inp["conf_ln_g"][l].reshape(2, 128).T
        lp[l, :, LP_LNB:LP_LNB + 2] = inp["conf_ln_b"][l].reshape(2, 128).T
        lp[l, :, LP_SCW:LP_SCW + 6] = inp["sc_conv_w"][l].reshape(3, 2, 128).transpose(2, 1, 0).reshape(128, 6)
        lp[l, :, LP_FFW:LP_FFW + 132] = inp["ffn_conv_w"][l].reshape(3, 44, 128).transpose(2, 1, 0).reshape(128, 132)
        rowp[l, 0, 0:1024] = inp["norm_mix_pre"][l]
        rowp[l, 0, 1024:2048] = inp["norm_mix_post"][l]
        rowp[l, 0, 2048:3072] = inp["norm_ffn_pre"][l]
        rowp[l, 0, 3072:4096] = inp["norm_ffn_post"][l]
        rowp[l, 0, 4096:4352] = inp["ret_norm_g"][l]
    return lp, rowp


_CACHE = {}


def run(inp, T, NL, ncores, dbg=False, trace=False, same_engine_sync=True):
    key = (T, NL, dbg, same_engine_sync)
    if key not in _CACHE:
        _CACHE[key] = build(T, NL, dbg, same_engine_sync)
    nc, S = _CACHE[key]
    f = lambda a: np.ascontiguousarray(np.asarray(a, dtype=np.float32))
    lp, rowp = host_layout({k: np.asarray(v) for k, v in inp.items()}, NL)
    con, cs = host_consts(T)
    shared = {
        "w_in": f(inp["w_in"][:NL]), "w_out": f(inp["w_out"][:NL]), "ffn_up": f(inp["ffn_up"][:NL]),
        "ffn_down": f(inp["ffn_down"][:NL]), "lp": lp, "rowp": rowp, "consts": con, "cs": cs,
    }
    x = np.asarray(inp["x"], dtype=np.float32)
    maps = [dict(shared, x=np.ascontiguousarray(x[b, :T])) for b in range(ncores)]
    res = run_bass_kernel_spmd(nc, maps, core_ids=list(range(ncores)), trace=trace)
    return res


def kernel(**inputs):
    res = run(inputs, 4096, 4, 8)
    return np.stack([r["y"] for r in res.results], 0).astype(np.float32)
```

```python
import concourse.bass as bass
import concourse.mybir as mybir

F32 = mybir.dt.float32
BF16 = mybir.dt.bfloat16
AF = mybir.ActivationFunctionType
ALU = mybir.AluOpType
AX = mybir.AxisListType

ENGS = ("pe", "act", "dve", "pool", "sp")


class Buf:
    __slots__ = ("name", "w", "r")

    def __init__(self, name):
        self.name = name
        self.w = []
        self.r = []


class V:
    __slots__ = ("ap", "bufs")

    def __init__(self, ap, bufs):
        self.ap = ap
        self.bufs = tuple(bufs)

    def __getitem__(self, idx):
        return V(self.ap[idx], self.bufs)

    def bc(self, shape):
        return V(self.ap.to_broadcast(shape), self.bufs)

    def re(self, pattern, **kw):
        return V(self.ap.rearrange(pattern, **kw), self.bufs)

    def wb(self, *bufs):
        return V(self.ap, bufs)


class Sched:
    def __init__(self, nc, n_dma_sems=20, same_engine_sync=True):
        self.nc = nc
        self.streams = {e: [] for e in ENGS}
        self.cnt = {e: 0 for e in ENGS}
        self.waited = {e: {} for e in ENGS}
        self.same_engine_sync = same_engine_sync
        self.n_dma_sems = n_dma_sems
        self.dma_val = [0] * n_dma_sems
        self.dma_next = 0
        self.n_ops = 0
        self.n_waits = 0
        self.n_sw = 0

    def _need(self, eng, waits, ev):
        key, val = ev
        if key == eng and (eng == "pe" or not self.same_engine_sync):
            return
        if self.waited[eng].get(key, 0) >= val:
            return
        if waits.get(key, 0) < val:
            waits[key] = val

    def _deps(self, eng, outs, ins, waits):
        for v in ins:
            for b in v.bufs:
                for ev in b.w:
                    self._need(eng, waits, ev)
        for v in outs:
            for b in v.bufs:
                for ev in b.w:
                    self._need(eng, waits, ev)
                for ev in b.r:
                    self._need(eng, waits, ev)

    def _commit(self, eng, waits, outs, ins, ev):
        for k, val in waits.items():
            self.waited[eng][k] = max(self.waited[eng].get(k, 0), val)
        for v in ins:
            for b in v.bufs:
                b.r.append(ev)
        for v in outs:
            for b in v.bufs:
                b.w = [ev]
                b.r = []

    def op(self, eng, fn, outs, ins):
        waits = {}
        self._deps(eng, outs, ins, waits)
        self.cnt[eng] += 1
        ev = (eng, self.cnt[eng])
        self._commit(eng, waits, outs, ins, ev)
        self.streams[eng].append((waits, fn, (eng, 1)))
        self.n_ops += 1
        self.n_waits += len(waits)

    def dma(self, q, out, in_, **kw):
        waits = {}
        self._deps(q, [out], [in_], waits)
        if q == "pool":
            key = ("sw", self.n_sw)
            self.n_sw += 1
            ev = (key, 16)
        else:
            k = self.dma_next
            self.dma_next = (k + 1) % self.n_dma_sems
            key = ("dma", k)
            if self.dma_val[k] > 0:
                self._need(q, waits, (key, self.dma_val[k]))
            self.dma_val[k] += 16
            ev = (key, self.dma_val[k])
        self._commit(q, waits, [out], [in_], ev)
        o_ap, i_ap = out.ap, in_.ap
        self.streams[q].append(
            (waits, lambda e: e.dma_start(out=o_ap, in_=i_ap, **kw), (key, 16)))
        self.n_ops += 1
        self.n_waits += len(waits)
        return ev

    def barrier(self):
        evs = [(e, self.cnt[e]) for e in ENGS if self.cnt[e] > 0]
        evs += [(("dma", k), v) for k, v in enumerate(self.dma_val) if v > 0]
        evs += [(("sw", k), 16) for k in range(self.n_sw)]
        for e in ENGS:
            waits = {}
            for ev in evs:
                if ev[0] == e:
                    continue
                self._need(e, waits, ev)
            if waits:
                for k, val in waits.items():
                    self.waited[e][k] = max(self.waited[e].get(k, 0), val)
                self.streams[e].append((waits, None, None))

    def emit(self, final_events=()):
        nc = self.nc
        from contextlib import ExitStack
        with ExitStack() as es:
            sems = {}
            for e in ENGS:
                sems[e] = es.enter_context(nc.semaphore("s_" + e))
            for k in range(self.n_dma_sems):
                sems[("dma", k)] = es.enter_context(nc.semaphore("s_dma%d" % k))
            for k in range(self.n_sw):
                sems[("sw", k)] = es.enter_context(nc.semaphore("s_sw%d" % k))
            block = es.enter_context(nc.Block())
            streams = self.streams

            def run(engname, eng):
                for waits, fn, inc in streams[engname]:
                    for k, val in waits.items():
                        eng.wait_ge(sems[k], val)
                    if fn is None:
                        continue
                    ins = fn(eng)
                    ins.then_inc(sems[inc[0]], inc[1])

            @block.tensor
            def _(e):
                run("pe", e)

            @block.scalar
            def _(e):
                run("act", e)

            @block.vector
            def _(e):
                run("dve", e)

            @block.gpsimd
            def _(e):
                run("pool", e)

            @block.sync
            def _(e):
                run("sp", e)
                for k in range(self.n_dma_sems):
                    if self.dma_val[k] > 0:
                        e.wait_ge(sems[("dma", k)], self.dma_val[k])
                for k in range(self.n_sw):
                    e.wait_ge(sems[("sw", k)], 16)

    def matmul(self, out, lhsT, rhs, start=True, stop=True, **kw):
        o, l, r = out.ap, lhsT.ap, rhs.ap
        self.op("pe", lambda e: e.matmul(o, l, r, start=start, stop=stop, **kw),
                [out], [lhsT, rhs])

    def transpose(self, out, in_, ident):
        o, i, d = out.ap, in_.ap, ident.ap
        self.op("pe", lambda e: e.transpose(o, i, d), [out], [in_, ident])

    def act(self, out, in_, func, bias=None, scale=None, accum_out=None):
        ins = [in_]
        outs = [out]
        kw = {}
        if bias is not None:
            if isinstance(bias, V):
                ins.append(bias)
                kw["bias"] = bias.ap
            else:
                kw["bias"] = float(bias)
        if scale is not None:
            if isinstance(scale, V):
                ins.append(scale)
                kw["scale"] = scale.ap
            else:
                kw["scale"] = float(scale)
        if accum_out is not None:
            outs.append(accum_out)
            kw["accum_out"] = accum_out.ap
        o, i = out.ap, in_.ap
        self.op("act", lambda e: e.activation(out=o, in_=i, func=func, **kw), outs, ins)

    def tt(self, eng, out, in0, in1, op):
        o, a, b = out.ap, in0.ap, in1.ap
        self.op(eng, lambda e: e.tensor_tensor(out=o, in0=a, in1=b, op=op), [out], [in0, in1])

    def ts(self, eng, out, in0, s1, op0, s2=None, op1=None):
        ins = [in0]
        a1 = s1.ap if isinstance(s1, V) else float(s1)
        if isinstance(s1, V):
            ins.append(s1)
        a2 = None
        if s2 is not None:
            a2 = s2.ap if isinstance(s2, V) else float(s2)
            if isinstance(s2, V):
                ins.append(s2)
        o, a = out.ap, in0.ap
        if op1 is None:
            self.op(eng, lambda e: e.tensor_scalar(out=o, in0=a, scalar1=a1, scalar2=a2, op0=op0),
                    [out], ins)
        else:
            self.op(eng, lambda e: e.tensor_scalar(out=o, in0=a, scalar1=a1, scalar2=a2, op0=op0, op1=op1),
                    [out], ins)

    def stt(self, out, in0, scalar, in1, op0, op1):
        ins = [in0, in1]
        sc = scalar.ap if isinstance(scalar, V) else float(scalar)
        if isinstance(scalar, V):
            ins.append(scalar)
        o, a, b = out.ap, in0.ap, in1.ap
        self.op("dve", lambda e: e.scalar_tensor_tensor(out=o, in0=a, scalar=sc, in1=b, op0=op0, op1=op1),
                [out], ins)

    def copy(self, eng, out, in_):
        o, i = out.ap, in_.ap
        if eng == "act":
            self.op("act", lambda e: e.copy(out=o, in_=i), [out], [in_])
        else:
            self.op(eng, lambda e: e.tensor_copy(out=o, in_=i), [out], [in_])

    def reduce(self, eng, out, in_, op, axis=AX.X):
        o, i = out.ap, in_.ap
        self.op(eng, lambda e: e.tensor_reduce(out=o, in_=i, axis=axis, op=op), [out], [in_])

    def memset(self, eng, out, val):
        o = out.ap
        self.op(eng, lambda e: e.memset(o, val), [out], [])

import numpy as np
from contextlib import ExitStack
from concourse.bass_utils import run_bass_kernel_spmd

DM = 1024
DIN = 3072
DFF = 2816
NFT = 22
TT = 256
EPS = 1e-6
NLP = 62 + 2 + 2 + 2 + 6 + 132
LP_DWW, LP_DWB, LP_LNG, LP_LNB, LP_SCW, LP_FFW = 0, 62, 64, 66, 68, 74
NROW = 4 * 1024 + 256
C_ID, C_M1, C_CAUS, C_MASK, C_QKD, C_CD = 0, 128, 256, 384, 384 + 512, 384 + 512 + 8
NCON = C_CD + 2


def host_consts(T):
    c = np.zeros((128, NCON), np.float32)
    j = np.arange(128)
    c[:, C_ID:C_ID + 128] = np.eye(128)
    c[:, C_M1:C_M1 + 128] = -(j[:, None] >= j[None, :]).astype(np.float32)
    c[:, C_CAUS:C_CAUS + 128] = (j[None, :] >= j[:, None]).astype(np.float32)
    t = np.arange(256)
    for r in range(2):
        c[:, C_MASK + r * 256:C_MASK + (r + 1) * 256] = ((128 * r + j)[:, None] < t[None, :]).astype(np.float32)
    lg = np.log1p(-np.exp2(-5.0 - np.arange(4, dtype=np.float64)))
    for h in range(4):
        c[:, C_QKD + h] = np.exp((j + 1.0) * lg[h])
        c[:, C_QKD + 4 + h] = np.exp(-(j + 1.0) * lg[h]) * 0.125
    for idx in range(2):
        for half in range(2):
            c[half * 64:(half + 1) * 64, C_CD + idx] = np.exp(128.0 * lg[2 * idx + half])
    pos = np.arange(T, dtype=np.float32)
    inv = (10000.0 ** (-np.arange(0, 64, 2, dtype=np.float32) / 64)).astype(np.float32)
    ang = (pos[:, None] * inv[None, :]).astype(np.float32)
    cs = np.concatenate([np.cos(ang), np.sin(ang)], -1).astype(np.float32)
    cs = np.ascontiguousarray(cs.reshape(T // 128, 128, 64).transpose(1, 0, 2))
    return c, cs


def build(T, NL, dbg=False, same_engine_sync=True):
    import os
    KSTOP = int(os.environ.get("KSTOP", "99"))
    KNOFFN = int(os.environ.get("KNOFFN", "0"))
    NT = T // TT
    NB = T // 128
    nc = bass.Bass("TRN2", target_bir_lowering=False)
    dram = lambda n, s, k="ExternalInput", dt=F32: nc.dram_tensor(n, list(s), dt, kind=k).ap()
    x_d = dram("x", [T, DM])
    win_d = dram("w_in", [NL, DM, DIN])
    wout_d = dram("w_out", [NL, DM, DM])
    wup_d = dram("ffn_up", [NL, DM, 2 * DFF])
    wdn_d = dram("ffn_down", [NL, DFF, DM])
    lp_d = dram("lp", [NL, 128, NLP])
    rowp_d = dram("rowp", [NL, 1, NROW])
    con_d = dram("consts", [128, NCON])
    cs_d = dram("cs", [128, NB, 64])
    y_d = dram("y", [T, DM], "ExternalOutput")
    xa_d = dram("xa", [T, DM], "Internal")
    xb_d = dram("xb", [T, DM], "Internal")
    if dbg:
        dmix_d = dram("dbg_mix", [NT, 128, 8, TT], "ExternalOutput", BF16)
        dxa_d = dram("dbg_xa", [T, DM], "ExternalOutput")

    S = Sched(nc, same_engine_sync=same_engine_sync)
    xbufs = {}

    def xview(ap, name, tt, s):
        bl = xbufs.setdefault(name, [Buf("%s%d" % (name, i)) for i in range(NT)])
        r0 = tt * TT + s * 128
        return V(ap[r0:r0 + 128, :], [bl[tt]])

    with ExitStack() as top:
        uid = [0]

        def sbt(es, name, shape, dt):
            uid[0] += 1
            return es.enter_context(nc.sbuf_tensor("%s_u%d" % (name, uid[0]), list(shape), dt))

        def alloc(es, name, shape, dt):
            t = sbt(es, name, shape, dt)
            return V(t[:], [Buf(name)])

        banks = []
        for i in range(8):
            t = top.enter_context(nc.psum_tensor("bank%d" % i, [128, 512], F32))
            banks.append(V(t[:], [Buf("bank%d" % i)]))

        class Rot:
            def __init__(self, items):
                self.items = items
                self.i = 0

            def get(self):
                v = self.items[self.i % len(self.items)]
                self.i += 1
                return v

        def bfview(bank):
            return V(bank.ap.bitcast(BF16), bank.bufs)

        con = alloc(top, "con", [128, NCON], F32)
        S.dma("sp", con, V(con_d, [Buf("con_d")]))
        identb = alloc(top, "identb", [128, 128], BF16)
        negm1 = alloc(top, "negm1", [128, 128], BF16)
        negones = alloc(top, "negones", [128, 128], BF16)
        onesm = alloc(top, "onesm", [128, 128], BF16)
        S.copy("dve", identb, con[:, C_ID:C_ID + 128])
        S.copy("dve", negm1, con[:, C_M1:C_M1 + 128])
        S.memset("dve", negones, -1.0)
        S.memset("dve", onesm, 1.0 / 256)
        identf = con[:, C_ID:C_ID + 128]
        caus = con[:, C_CAUS:C_CAUS + 128]
        maskd = [con[:, C_MASK + r * 256:C_MASK + (r + 1) * 256] for r in range(2)]
        qkd = con[:, C_QKD:C_QKD + 8]
        cdt = con[:, C_CD:C_CD + 2]
        cs_dv = V(cs_d, [Buf("cs_d")])

        def norm_transpose(tt, srcname, src_ap, xs, ssv, rs, junk, hbs, hT, gpre, gen):
            for s in range(2):
                S.dma("sp", xs[s], xview(src_ap, srcname, tt, s))
                S.act(hbs[s], xs[s], AF.Square, accum_out=ssv[s])
                S.act(rs[s], ssv[s], AF.Ln, scale=1.0 / DM, bias=EPS)
                S.act(rs[s], rs[s], AF.Exp, scale=-0.5)
                S.stt(hbs[s], xs[s], rs[s], gpre, ALU.mult, ALU.mult)
                pb = bfview(gen.get())
                for kc in range(8):
                    S.transpose(pb[:, kc * 128:(kc + 1) * 128], hbs[s][:, kc * 128:(kc + 1) * 128], identb)
                S.copy("act" if s == 0 else "dve", hT[:, :, s * 128:(s + 1) * 128],
                       pb.re("p (a b) -> p a b", b=128))

        def post_residual(tt, s, bk, srcname, src_ap, dstname, dst_ap, xs, junk, ssp, ssum, rs2, gpost, tmp):
            for g in range(2):
                S.act(junk[:, 0:512], bk[g], AF.Square, accum_out=ssp[:, g:g + 1])
            S.tt("dve", ssum, ssp[:, 0:1], ssp[:, 1:2], ALU.add)
            S.act(rs2, ssum, AF.Ln, scale=1.0 / DM, bias=EPS)
            S.act(rs2, rs2, AF.Exp, scale=-0.5)
            S.dma("sp", xs[s], xview(src_ap, srcname, tt, s))
            for g in range(2):
                S.stt(tmp[:, g * 512:(g + 1) * 512], bk[g], rs2, gpost[:, g * 512:(g + 1) * 512],
                      ALU.mult, ALU.mult)
            S.tt("pool", tmp, tmp, xs[s], ALU.add)
            S.dma("sp", xview(dst_ap, dstname, tt, s), tmp)

        def mixer_phase(l, srcname, src_ap, dstname, dst_ap):
            with ExitStack() as es:
                A_ = lambda n, s, d: alloc(es, n, s, d)
                win_t = sbt(es, "win", [128, 8, DIN], BF16)
                wb_ = [Buf("winA"), Buf("winB")]
                win = [V(win_t[:][:, kc, :], [wb_[kc // 4]]) for kc in range(8)]
                wout_t = sbt(es, "wout", [128, 8, DM], BF16)
                wob_ = Buf("woutA")
                wout = [V(wout_t[:][:, kc, :], [wob_]) for kc in range(8)]
                lp = A_("lp", [128, NLP], F32)
                gpre = A_("gpre", [128, DM], F32)
                gpost = A_("gpost", [128, DM], F32)
                retg = A_("retg", [128, 256], F32)
                diag = A_("diag", [128, 62, 128], BF16)
                rowv = V(rowp_d[l], [Buf("rowp_d")])
                S.dma("sp", lp, V(lp_d[l], [Buf("lp_d")]))
                S.dma("sp", gpre, rowv[:, 0:1024].bc([128, 1024]))
                S.dma("sp", gpost, rowv[:, 1024:2048].bc([128, 1024]))
                S.dma("sp", retg, rowv[:, 4096:4352].bc([128, 256]))
                wv = V(win_d[l].rearrange("(kc p) n -> p kc n", p=128), [Buf("win_d")])
                for g in range(2):
                    S.dma("pool", V(win_t[:][:, 4 * g:4 * g + 4, :], [wb_[g]]), wv[:, 4 * g:4 * g + 4, :])
                wv = V(wout_d[l].rearrange("(kc p) n -> p kc n", p=128), [Buf("wout_d")])
                S.dma("pool", V(wout_t[:], [wob_]), wv)
                for i in range(62):
                    S.ts("dve" if i % 2 else "pool", diag[:, i, :], identf, lp[:, LP_DWW + i:LP_DWW + i + 1], ALU.mult)

                xs = [A_("xs%d" % s, [128, DM], F32) for s in range(2)]
                hbs = [A_("hbs%d" % s, [128, DM], BF16) for s in range(2)]
                ssv = [A_("ssv%d" % s, [128, 1], F32) for s in range(2)]
                rs = [A_("rs%d" % s, [128, 1], F32) for s in range(2)]
                junk = A_("junk", [128, 512], BF16)
                hT = A_("hT", [128, 8, TT], BF16)
                cst = A_("cst", [128, 2, 64], F32)
                kT_t = sbt(es, "kT", [128, 2, T], BF16)
                kTb = [Buf("kT%d" % i) for i in range(NT)]
                vc_t = sbt(es, "vc", [128, NB, 256], BF16)
                vcb = [Buf("vc%d" % i) for i in range(NT)]
                qTs = [A_("qT%d" % i, [128, 2, TT], BF16) for i in range(2)]
                xsB1 = A_("xsB", [128, DM], F32)
                xsB = [xsB1, xsB1]
                Ub = [[A_("Ub%d%d" % (ct, p), [128, 30 + TT], BF16) for p in range(2)] for ct in range(2)]
                sgm = A_("sgm", [128, TT], F32)
                cvb = A_("cvb", [128, 2, TT], F32)
                cvh = A_("cvh", [128, 2, TT], BF16)
                sqb = A_("sqb", [128, 2, TT], BF16)
                mean_sb = A_("mean_sb", [128, TT], F32)
                var_sb = A_("var_sb", [128, TT], F32)
                ytmp = A_("ytmp", [128, TT], F32)
                Csb = A_("Csb", [128, TT], F32)
                Bsb = A_("Bsb", [128, 2, TT], BF16)
                Pb = [A_("Pb%d" % ct, [128, 2 + TT], F32) for ct in range(2)]
                tcv = A_("tcv", [128, TT], F32)
                r32 = A_("r32", [128, 512], F32)
                rot = A_("rot", [128, 512], F32)
                rta = A_("rta", [128, 256], F32)
                rtb = A_("rtb", [128, 256], F32)
                rtc = A_("rtc", [128, 256], F32)
                rtd = A_("rtd", [128, 256], F32)
                rqkb = A_("rqkb", [128, 512], BF16)
                rqkT = A_("rqkT", [128, 4, 128], BF16)
                rvb = A_("rvb", [128, 256], BF16)
                sgate = A_("sgate", [128, 256], F32)
                innD = A_("innD", [128, 512], BF16)
                osb = A_("osb", [128, 256], F32)
                osq = A_("osq", [128, 256], F32)
                st4 = [A_("st4_%d" % i, [128, 4], F32) for i in range(4)]
                yn = A_("yn", [128, 256], F32)
                ybf = A_("ybf", [128, 256], BF16)
                S32 = A_("S32", [128, 2, 64], F32)
                Sbf = A_("Sbf", [128, 2, 64], BF16)
                e_ = [[A_("e%d_%d" % (i, h), [128, TT], F32) for h in range(2)] for i in range(3)]
                sp_ = [[A_("sp%d_%d" % (i, h), [128, TT], BF16) for h in range(2)] for i in range(2)]
                Aacc = [A_("Aacc%d" % h, [128, TT], BF16) for h in range(2)]
                E2 = [A_("E2_%d" % h, [128, TT], F32) for h in range(2)]
                attn = [[A_("attn%d_%d" % (i, h), [128, TT], BF16) for h in range(2)] for i in range(2)]
                mixTs = [A_("mixT%d" % i, [128, 8, TT], BF16) for i in range(2)]
                ssp = A_("ssp", [128, 2], F32)
                ssum = A_("ssum", [128, 1], F32)
                rs2 = A_("rs2", [128, 1], F32)
                tmp1 = A_("tmp", [128, DM], F32)
                tmp = [tmp1, tmp1]

                gen = Rot(banks[0:2])
                zsl = [banks[2 + hh][:, 0:TT] for hh in range(2)]
                tsl = [banks[4 + hh][:, 0:TT] for hh in range(2)]
                posl = [banks[6 + hh][hh * 64:hh * 64 + 64, :] for hh in range(2)]
                for ct in range(2):
                    S.memset("pool", Ub[ct][1][:, TT:TT + 30], 0.0)
                    S.memset("pool", Pb[ct][:, TT:TT + 2], 0.0)
                S.memset("pool", S32, 0.0)
                S.memset("pool", Sbf, 0.0)

                def proj_feat(dst, col0):
                    for kc in range(8):
                        S.matmul(dst, win[kc][:, col0:col0 + 128], hT[:, kc, :], start=(kc == 0), stop=(kc == 7))

                def stage_A(tt):
                    mT = mixTs[tt % 2]
                    qTt = qTs[tt % 2]
                    t0 = tt * TT
                    par = tt % 2
                    kTv = V(kT_t[:][:, :, t0:t0 + TT], [kTb[tt]])
                    norm_transpose(tt, srcname, src_ap, xs, ssv, rs, junk, hbs, hT, gpre, gen)
                    S.dma("sp", cst, cs_dv[:, 2 * tt:2 * tt + 2, :])
                    yield
                    for ct in range(2):
                        bk = gen.get()
                        proj_feat(bk[:, 0:TT], ct * 128)
                        proj_feat(bk[:, TT:2 * TT], 256 + ct * 128)
                        S.act(sgm, bk[:, TT:2 * TT], AF.Sigmoid)
                        S.copy("pool", Ub[ct][par][:, 0:30], Ub[ct][1 - par][:, TT:TT + 30])
                        S.tt("dve", Ub[ct][par][:, 30:30 + TT], bk[:, 0:TT], sgm, ALU.mult)
                        yield
                    yield
                    for ct in range(2):
                        bk = gen.get()
                        proj_feat(bk[:, 0:TT], 512 + ct * 128)
                        proj_feat(bk[:, TT:2 * TT], 768 + ct * 128)
                        S.ts("dve", qTt[:, ct, :], bk[:, 0:TT], 0.125, ALU.mult)
                        S.copy("dve", kTv[:, ct, :], bk[:, TT:2 * TT])
                        yield
                    yield
                    for ct in range(2):
                        bk = gen.get()
                        bk2 = gen.get()
                        proj_feat(bk[:, 0:TT], 2304 + ct * 128)
                        proj_feat(bk[:, TT:2 * TT], 2560 + ct * 128)
                        proj_feat(bk2[:, 0:TT], 2816 + ct * 128)
                        S.copy("act", Bsb[:, ct, :], bk[:, 0:TT])
                        S.copy("act", Csb, bk[:, TT:2 * TT])
                        S.copy("pool", Pb[ct][:, 0:2], Pb[ct][:, TT:TT + 2])
                        S.tt("dve", Pb[ct][:, 2:2 + TT], bk2[:, 0:TT], Csb, ALU.mult)
                        yield
                    yield
                    for ct in range(2):
                        bk = gen.get()
                        for k in range(31):
                            S.matmul(bk[:, 0:TT], diag[:, ct * 31 + k, :], Ub[ct][par][:, k:k + TT],
                                     start=(k == 0), stop=(k == 30))
                        S.ts("dve", cvb[:, ct, :], bk[:, 0:TT], lp[:, LP_DWB + ct:LP_DWB + ct + 1], ALU.add)
                        S.act(sqb[:, ct, :], cvb[:, ct, :], AF.Square)
                        S.copy("dve", cvh[:, ct, :], cvb[:, ct, :])
                        yield
                    bk = gen.get()
                    for ct in range(2):
                        S.matmul(bk[:, 0:TT], onesm, cvh[:, ct, :], start=(ct == 0), stop=(ct == 1))
                    for ct in range(2):
                        S.matmul(bk[:, TT:2 * TT], onesm, sqb[:, ct, :], start=(ct == 0), stop=(ct == 1))
                    S.copy("act", mean_sb, bk[:, 0:TT])
                    S.tt("dve", var_sb, mean_sb, mean_sb, ALU.mult)
                    S.tt("dve", var_sb, bk[:, TT:2 * TT], var_sb, ALU.subtract)
                    S.act(var_sb, var_sb, AF.Ln, bias=EPS)
                    S.act(var_sb, var_sb, AF.Exp, scale=-0.5)
                    for ct in range(2):
                        S.tt("dve", ytmp, cvb[:, ct, :], mean_sb, ALU.subtract)
                        S.tt("dve", ytmp, ytmp, var_sb, ALU.mult)
                        S.act(mT[:, ct, :], ytmp, AF.Silu, scale=lp[:, LP_LNG + ct:LP_LNG + ct + 1],
                              bias=lp[:, LP_LNB + ct:LP_LNB + ct + 1])
                    yield
                    for ct in range(2):
                        w = lambda k: lp[:, LP_SCW + ct * 3 + k:LP_SCW + ct * 3 + k + 1]
                        S.ts("dve", tcv, Pb[ct][:, 2:2 + TT], w(2), ALU.mult)
                        S.stt(tcv, Pb[ct][:, 1:1 + TT], w(1), tcv, ALU.mult, ALU.add)
                        S.stt(tcv, Pb[ct][:, 0:TT], w(0), tcv, ALU.mult, ALU.add)
                        S.tt("dve", mT[:, 6 + ct, :], tcv, Bsb[:, ct, :], ALU.mult)
                    yield
                    for s in range(2):
                        cb = tt * 2 + s
                        tok = slice(s * 128, (s + 1) * 128)
                        bA = gen.get()
                        for kc in range(8):
                            S.matmul(bA, hT[:, kc, tok], win[kc][:, 1280:1792], start=(kc == 0), stop=(kc == 7))
                        S.copy("act", r32, bA)
                        bB = gen.get()
                        for kc in range(8):
                            S.matmul(bB, hT[:, kc, tok], win[kc][:, 1792:2304], start=(kc == 0), stop=(kc == 7))
                        S.copy("act", rvb, bB[:, 0:256])
                        S.act(sgate, bB[:, 256:512], AF.Silu)
                        bC = gen.get()
                        for kc in range(8):
                            S.matmul(bC[:, 0:256], hT[:, kc, tok], win[kc][:, 1024:1280], start=(kc == 0), stop=(kc == 7))
                        S.copy("act", V(vc_t[:][:, cb, :], [vcb[tt]]), bC[:, 0:256])
                        yield
                        xv = r32.re("p (g two f) -> p g two f", two=2, f=32)
                        ov = rot.re("p (g two f) -> p g two f", two=2, f=32)
                        x1, x2 = xv[:, :, 0, :], xv[:, :, 1, :]
                        cosb = cst[:, s, 0:32].re("p (o f) -> p o f", o=1).bc([128, 8, 32])
                        sinb = cst[:, s, 32:64].re("p (o f) -> p o f", o=1).bc([128, 8, 32])
                        v3 = lambda t: t.re("p (g f) -> p g f", f=32)
                        S.tt("dve", v3(rta), x1, cosb, ALU.mult)
                        S.tt("dve", v3(rtb), x2, sinb, ALU.mult)
                        S.tt("dve", ov[:, :, 0, :], v3(rta), v3(rtb), ALU.subtract)
                        S.tt("pool", v3(rtc), x2, cosb, ALU.mult)
                        S.tt("pool", v3(rtd), x1, sinb, ALU.mult)
                        S.tt("pool", ov[:, :, 1, :], v3(rtc), v3(rtd), ALU.add)
                        S.tt("dve", rqkb.re("p (g f) -> p g f", f=64), rot.re("p (g f) -> p g f", f=64),
                             qkd.re("p (g o) -> p g o", o=1).bc([128, 8, 64]), ALU.mult)
                        pb = bfview(gen.get())
                        for j in range(4):
                            S.transpose(pb[:, j * 128:(j + 1) * 128], rqkb[:, j * 128:(j + 1) * 128], identb)
                        S.copy("act", rqkT, pb[:, 0:512].re("p (a b) -> p a b", b=128))
                        yield
                        bI = [gen.get(), gen.get()]
                        for h in range(4):
                            pr = slice((h % 2) * 64, (h % 2) * 64 + 64)
                            S.matmul(bI[h % 2][:, (h // 2) * 128:(h // 2) * 128 + 128], rqkT[pr, 2 + h // 2, :],
                                     rqkT[pr, h // 2, :])
                        for par2 in range(2):
                            S.tt("dve", innD[:, par2 * 256:(par2 + 1) * 256].re("p (h q) -> p h q", q=128),
                                 bI[par2][:, 0:256].re("p (h q) -> p h q", q=128),
                                 caus.re("p (o q) -> p o q", o=1).bc([128, 2, 128]), ALU.mult)
                        bO = [gen.get(), gen.get()]
                        for h in range(4):
                            pr = slice((h % 2) * 64, (h % 2) * 64 + 64)
                            j = (h % 2) * 2 + h // 2
                            dst = bO[h % 2][:, (h // 2) * 64:(h // 2) * 64 + 64]
                            S.matmul(dst, innD[:, j * 128:(j + 1) * 128],
                                     rvb[:, h * 64:(h + 1) * 64], start=True, stop=False)
                            S.matmul(dst, rqkT[pr, h // 2, :], Sbf[pr, h // 2, :],
                                     start=False, stop=True)
                        for par2 in range(2):
                            S.copy("act", osb.re("p (a b e) -> p a b e", b=2, e=64)[:, :, par2, :],
                                   bO[par2][:, 0:128].re("p (a e) -> p a e", e=64))
                        bS = gen.get()
                        for h in range(4):
                            pr = slice((h % 2) * 64, (h % 2) * 64 + 64)
                            S.matmul(bS[pr, (h // 2) * 64:(h // 2) * 64 + 64], rqkb[:, 256 + h * 64:256 + (h + 1) * 64],
                                     rvb[:, h * 64:(h + 1) * 64])
                        S.tt("dve", S32, S32, bS[:, 0:128].re("p (a e) -> p a e", e=64), ALU.add)
                        S.tt("dve", S32, S32, cdt.re("p (a o) -> p a o", o=1).bc([128, 2, 64]), ALU.mult)
                        S.copy("dve", Sbf, S32)
                        yield
                        S.act(osq, osb, AF.Square)
                        o3 = lambda t: t.re("p (h e) -> p h e", e=64)
                        S.reduce("dve", st4[0], o3(osb), ALU.add)
                        S.reduce("dve", st4[1], o3(osq), ALU.add)
                        S.ts("dve", st4[0], st4[0], 1.0 / 64, ALU.mult)
                        S.tt("dve", st4[2], st4[0], st4[0], ALU.mult)
                        S.stt(st4[3], st4[1], 1.0 / 64, st4[2], ALU.mult, ALU.subtract)
                        S.act(st4[3], st4[3], AF.Ln, bias=EPS)
                        S.act(st4[3], st4[3], AF.Exp, scale=-0.5)
                        b4 = lambda t: t.re("p (h o) -> p h o", o=1).bc([128, 4, 64])
                        S.tt("dve", o3(yn), o3(osb), b4(st4[0]), ALU.subtract)
                        S.tt("dve", o3(yn), o3(yn), b4(st4[3]), ALU.mult)
                        S.tt("pool", yn, yn, retg, ALU.mult)
                        S.tt("pool", ybf, yn, sgate, ALU.mult)
                        pb = bfview(gen.get())
                        for j in range(2):
                            S.transpose(pb[:, j * 128:(j + 1) * 128], ybf[:, j * 128:(j + 1) * 128], identb)
                        S.copy("act", mT[:, 4:6, tok], pb[:, 0:256].re("p (a b) -> p a b", b=128))
                        yield

                def stage_B(tt):
                    mT = mixTs[tt % 2]
                    qTt = qTs[tt % 2]
                    nkb = 2 * tt + 2
                    kbs = list(range(nkb - 1, -1, -1))
                    for hp in range(2):
                        def st1a(n):
                            kb = kbs[n]
                            for hh in range(2):
                                pr = slice(hh * 64, hh * 64 + 64)
                                kblk = V(kT_t[:][pr, hp, kb * 128:(kb + 1) * 128], [kTb[kb // 2]])
                                S.matmul(zsl[hh], kblk, qTt[pr, hp, :])

                        def st1b(n):
                            for hh in range(2):
                                S.act(e_[n % 3][hh], zsl[hh], AF.Exp)

                        def st2(n):
                            kb = kbs[n]
                            r = kb - 2 * tt
                            first, last = (n == 0), (kb == 0)
                            for hh in range(2):
                                e, sp = e_[n % 3][hh], sp_[n % 2][hh]
                                S.act(sp, e, AF.Ln, bias=1.0)
                                if r >= 0:
                                    S.tt("pool", sp, sp, maskd[r], ALU.mult)
                                    S.tt("pool", e, e, maskd[r], ALU.mult)
                            for hh in range(2):
                                S.matmul(tsl[hh], negm1, sp_[n % 2][hh], start=True, stop=first)
                                if not first:
                                    S.matmul(tsl[hh], negones, Aacc[hh], start=False, stop=True)
                            if not last:
                                for hh in range(2):
                                    if first:
                                        S.copy("pool", Aacc[hh], sp_[n % 2][hh])
                                    else:
                                        S.tt("pool", Aacc[hh], Aacc[hh], sp_[n % 2][hh], ALU.add)

                        def st3(n):
                            kb = kbs[n]
                            first, last = (n == 0), (kb == 0)
                            for hh in range(2):
                                S.act(E2[hh], tsl[hh], AF.Exp)
                                S.tt("dve", attn[n % 2][hh], e_[n % 3][hh], E2[hh], ALU.mult)
                            for hh in range(2):
                                h = 2 * hp + hh
                                vblk = V(vc_t[:][:, kb, h * 64:(h + 1) * 64], [vcb[kb // 2]])
                                S.matmul(posl[hh][:, hp * TT:(hp + 1) * TT], vblk, attn[n % 2][hh],
                                         start=first, stop=last)

                        for itn in range(nkb + 2):
                            if itn < nkb:
                                st1a(itn)
                            if itn - 2 >= 0:
                                st3(itn - 2)
                            if 0 <= itn - 1 < nkb:
                                st2(itn - 1)
                            if itn < nkb:
                                st1b(itn)
                            yield
                        for hh in range(2):
                            pr = slice(hh * 64, hh * 64 + 64)
                            S.copy("act", mT[pr, 2 + hp, :], posl[hh][:, hp * TT:(hp + 1) * TT])
                    yield
                    if dbg:
                        S.dma("sp", V(dmix_d[tt], [Buf("dmix%d" % tt)]), mT)
                    for s in range(2):
                        tok = slice(s * 128, (s + 1) * 128)
                        bk = [gen.get(), gen.get()]
                        for g in range(2):
                            for kc in range(8):
                                S.matmul(bk[g], mT[:, kc, tok], wout[kc][:, g * 512:(g + 1) * 512],
                                         start=(kc == 0), stop=(kc == 7))
                        post_residual(tt, s, bk, srcname, src_ap, dstname, dst_ap, xsB, junk, ssp, ssum, rs2,
                                      gpost, tmp[s])
                        yield

                def drive(a, b):
                    while a is not None or b is not None:
                        if b is not None:
                            try:
                                next(b)
                            except StopIteration:
                                b = None
                        if a is not None:
                            try:
                                next(a)
                            except StopIteration:
                                a = None

                drive(stage_A(0), None)
                for tt in range(NT):
                    drive(stage_A(tt + 1) if tt + 1 < NT else None, stage_B(tt))
            S.barrier()

        def ffn_phase(l, srcname, src_ap, dstname, dst_ap):
            with ExitStack() as es:
                A_ = lambda n, s, d: alloc(es, n, s, d)
                wup_t = sbt(es, "wup", [128, 8, 2 * DFF], BF16)
                wub_ = [Buf("wup%d" % g) for g in range(4)]
                wup = [V(wup_t[:][:, kc, :], [wub_[kc // 2]]) for kc in range(8)]
                wdn_t = sbt(es, "wdn", [128, NFT, DM], BF16)
                wdb_ = [Buf("wdn%d" % g) for g in range(2)]
                wdn = [V(wdn_t[:][:, kc, :], [wdb_[kc // 11]]) for kc in range(NFT)]
                lp = A_("lp", [128, NLP], F32)
                gpre = A_("gpre", [128, DM], F32)
                gpost = A_("gpost", [128, DM], F32)
                rowv = V(rowp_d[l], [Buf("rowp_d")])
                S.dma("sp", lp, V(lp_d[l], [Buf("lp_d")]))
                S.dma("sp", gpre, rowv[:, 2048:3072].bc([128, 1024]))
                S.dma("sp", gpost, rowv[:, 3072:4096].bc([128, 1024]))
                wv = V(wup_d[l].rearrange("(kc p) n -> p kc n", p=128), [Buf("wup_d")])
                for g in range(4):
                    S.dma("pool", V(wup_t[:][:, 2 * g:2 * g + 2, :], [wub_[g]]), wv[:, 2 * g:2 * g + 2, :])
                wv = V(wdn_d[l].rearrange("(kc p) n -> p kc n", p=128), [Buf("wdn_d")])
                for g in range(2):
                    S.dma("pool", V(wdn_t[:][:, 11 * g:11 * g + 11, :], [wdb_[g]]), wv[:, 11 * g:11 * g + 11, :])
                xs = [A_("xs%d" % s, [128, DM], F32) for s in range(2)]
                hbs = [A_("hbs%d" % s, [128, DM], BF16) for s in range(2)]
                ssv = [A_("ssv%d" % s, [128, 1], F32) for s in range(2)]
                rs = [A_("rs%d" % s, [128, 1], F32) for s in range(2)]
                junk = A_("junk", [128, DM], BF16)
                hTs = [A_("hT%d" % i, [128, 8, TT], BF16) for i in range(2)]
                Bf = [A_("Bf%d" % i, [128, 2, TT + 2], F32) for i in range(2)]
                Hh = A_("Hh", [128, NFT, 2, 2], F32)
                tg = [A_("tg%d" % i, [128, TT], F32) for i in range(2)]
                tu = [A_("tu%d" % i, [128, TT], F32) for i in range(2)]
                sgt = [A_("sgt%d" % i, [128, TT], F32) for i in range(2)]
                actT = A_("actT", [128, NFT, TT], BF16)
                ssp = A_("ssp", [128, 2], F32)
                ssum = A_("ssum", [128, 1], F32)
                rs2 = A_("rs2", [128, 1], F32)
                tmp = [A_("tmp%d" % i, [128, DM], F32) for i in range(2)]
                gen = Rot(banks)
                S.memset("pool", Hh, 0.0)
                fw = lambda i, k: lp[:, LP_FFW + i * 3 + k:LP_FFW + i * 3 + k + 1]
                norm_transpose(0, srcname, src_ap, xs, ssv, rs, junk, hbs, hTs[0], gpre, gen)
                for tt in range(NT):
                    hT = hTs[tt % 2]
                    for i in range(NFT):
                        bk = gen.get()
                        B = Bf[i % 2]
                        for half, c0 in ((0, i * 128), (1, DFF + i * 128)):
                            for kc in range(8):
                                S.matmul(bk[:, half * TT:(half + 1) * TT], wup[kc][:, c0:c0 + 128], hT[:, kc, :],
                                         start=(kc == 0), stop=(kc == 7))
                        S.copy("pool", B[:, :, 0:2], Hh[:, i, :, :])
                        S.copy("act", B[:, :, 2:2 + TT], bk.re("p (a t) -> p a t", a=2))
                        S.copy("pool", Hh[:, i, :, :], B[:, :, TT:TT + 2])
                        g_, u_ = tg[i % 2], tu[i % 2]
                        S.ts("dve", g_, B[:, 0, 2:2 + TT], fw(i, 2), ALU.mult)
                        S.ts("dve", u_, B[:, 1, 2:2 + TT], fw(NFT + i, 2), ALU.mult)
                        S.stt(g_, B[:, 0, 1:1 + TT], fw(i, 1), g_, ALU.mult, ALU.add)
                        S.stt(u_, B[:, 1, 1:1 + TT], fw(NFT + i, 1), u_, ALU.mult, ALU.add)
                        S.stt(g_, B[:, 0, 0:TT], fw(i, 0), g_, ALU.mult, ALU.add)
                        S.stt(u_, B[:, 1, 0:TT], fw(NFT + i, 0), u_, ALU.mult, ALU.add)
                        S.act(sgt[i % 2], tg[i % 2], AF.Silu)
                        S.tt("pool", actT[:, i, :], sgt[i % 2], tu[i % 2], ALU.mult)
                    if tt + 1 < NT:
                        norm_transpose(tt + 1, srcname, src_ap, xs, ssv, rs, junk, hbs, hTs[(tt + 1) % 2], gpre, gen)
                    for s in range(2):
                        tok = slice(s * 128, (s + 1) * 128)
                        bk = [gen.get(), gen.get()]
                        for g in range(2):
                            for kc in range(NFT):
                                S.matmul(bk[g], actT[:, kc, tok], wdn[kc][:, g * 512:(g + 1) * 512],
                                         start=(kc == 0), stop=(kc == NFT - 1))
                        post_residual(tt, s, bk, srcname, src_ap, dstname, dst_ap, xs, junk, ssp, ssum, rs2,
                                      gpost, tmp[s])
            S.barrier()

        for l in range(NL):
            srcn, srca = ("x", x_d) if l == 0 else ("xb", xb_d)
            mixer_phase(l, srcn, srca, "xa", xa_d)
            if dbg and l == 0 and not int(os.environ.get('KNODX', '0')):
                for tt in range(NT):
                    for s in range(2):
                        r0 = tt * TT + s * 128
                        S.dma("sp", V(dxa_d[r0:r0 + 128, :], [Buf("dxa%d_%d" % (tt, s))]), xview(xa_d, "xa", tt, s))
            dstn, dsta = ("y", y_d) if l == NL - 1 else ("xb", xb_d)
            if not KNOFFN:
                ffn_phase(l, "xa", xa_d, dstn, dsta)
        S.emit()
    return nc, S


def host_layout(inp, NL):
    lp = np.zeros((NL, 128, NLP), np.float32)
    rowp = np.zeros((NL, 1, NROW), np.float32)
    for l in range(NL):
        lp[l, :, LP_DWW:LP_DWW + 62] = inp["conf_dw_w"][l].reshape(31, 2, 128).transpose(2, 1, 0).reshape(128, 62)
        lp[l, :, LP_DWB:LP_DWB + 2] = inp["conf_dw_b"][l].reshape(2, 128).T
        lp[l, :, LP_LNG:LP_LNG + 2] = inp["conf_ln_g"][l].reshape(2, 128).T
        lp[l, :, LP_LNB:LP_LNB + 2] = inp["conf_ln_b"][l].reshape(2, 128).T
        lp[l, :, LP_SCW:LP_SCW + 6] = inp["sc_conv_w"][l].reshape(3, 2, 128).transpose(2, 1, 0).reshape(128, 6)
        lp[l, :, LP_FFW:LP_FFW + 132] = inp["ffn_conv_w"][l].reshape(3, 44, 128).transpose(2, 1, 0).reshape(128, 132)
        rowp[l, 0, 0:1024] = inp["norm_mix_pre"][l]
        rowp[l, 0, 1024:2048] = inp["norm_mix_post"][l]
        rowp[l, 0, 2048:3072] = inp["norm_ffn_pre"][l]
        rowp[l, 0, 3072:4096] = inp["norm_ffn_post"][l]
        rowp[l, 0, 4096:4352] = inp["ret_norm_g"][l]
    return lp, rowp


_CACHE = {}


def run(inp, T, NL, ncores, dbg=False, trace=False, same_engine_sync=True):
    key = (T, NL, dbg, same_engine_sync)
    if key not in _CACHE:
        _CACHE[key] = build(T, NL, dbg, same_engine_sync)
    nc, S = _CACHE[key]
    f = lambda a: np.ascontiguousarray(np.asarray(a, dtype=np.float32))
    lp, rowp = host_layout({k: np.asarray(v) for k, v in inp.items()}, NL)
    con, cs = host_consts(T)
    shared = {
        "w_in": f(inp["w_in"][:NL]), "w_out": f(inp["w_out"][:NL]), "ffn_up": f(inp["ffn_up"][:NL]),
        "ffn_down": f(inp["ffn_down"][:NL]), "lp": lp, "rowp": rowp, "consts": con, "cs": cs,
    }
    x = np.asarray(inp["x"], dtype=np.float32)
    maps = [dict(shared, x=np.ascontiguousarray(x[b, :T])) for b in range(ncores)]
    res = run_bass_kernel_spmd(nc, maps, core_ids=list(range(ncores)), trace=trace)
    return res


def kernel(**inputs):
    res = run(inputs, 4096, 4, 8)
    return np.stack([r["y"] for r in res.results], 0).astype(np.float32)
```
